# Optimizing a Trainium2 kernel written in Bass

```python
import jax, jax.numpy as jnp
from jax import lax
import numpy as np

D_MODEL = 2048
BATCH = 4
SEQ = 4096
DEPTH = 1

MLSTM_HEADS = 8
MLSTM_QK_DIM = 128
MLSTM_V_DIM = 256
MLSTM_QK_WIDTH = MLSTM_HEADS * MLSTM_QK_DIM
MLSTM_V_WIDTH = MLSTM_HEADS * MLSTM_V_DIM
CHUNK = 64
CONV_WIDTH = D_MODEL
CONV_GROUPS = 16
CONV_K = 3
EPS = 1e-6

SPLIT_WIDTHS = (
    MLSTM_QK_WIDTH, MLSTM_QK_WIDTH,
    MLSTM_V_WIDTH, MLSTM_V_WIDTH, MLSTM_V_WIDTH,
    MLSTM_HEADS, MLSTM_HEADS,
    CONV_WIDTH, CONV_WIDTH, CONV_WIDTH, CONV_WIDTH,
    D_MODEL, D_MODEL,
)
IN_WIDTH = sum(SPLIT_WIDTHS)

kernel_name = "hybrid_mlstm_shortconv_gated_block"


def _split_offsets():
    offs, acc = [], 0
    for w in SPLIT_WIDTHS:
        offs.append(acc)
        acc += w
    return offs


def rms_norm(x, w):
    xf = x.astype(jnp.float32)
    y = xf * lax.rsqrt(jnp.mean(xf * xf, axis=-1, keepdims=True) + EPS)
    return (y * w.astype(jnp.float32)).astype(x.dtype)


def mlstm_chunkwise(q, k, v, i_pre, f_pre):
    f32 = jnp.float32
    bsz, s, h, dk = q.shape
    dv = v.shape[-1]
    nc = s // CHUNK

    def to_chunks(t):
        t = t.astype(f32).reshape((bsz, nc, CHUNK, h) + t.shape[3:])
        return jnp.moveaxis(t, (1, 3), (0, 2))

    qc = to_chunks(q) * (dk ** -0.5)
    kc = to_chunks(k)
    vc = to_chunks(v)
    log_i = to_chunks(i_pre)
    log_f = to_chunks(jax.nn.log_sigmoid(f_pre.astype(f32)))
    causal = jnp.tril(jnp.ones((CHUNK, CHUNK), dtype=bool))

    def step(carry, inp):
        c_state, n_state, m_state = carry
        qb, kb, vb, li, lf = inp
        b = jnp.cumsum(lf, axis=-1)
        g = b[..., -1]
        d_intra = b[..., :, None] - b[..., None, :] + li[..., None, :]
        d_intra = jnp.where(causal, d_intra, -jnp.inf)
        d_inter = b + m_state[..., None]
        m_row = jnp.maximum(d_inter, jnp.max(d_intra, axis=-1))
        w_intra = jnp.exp(d_intra - m_row[..., None])
        w_inter = jnp.exp(d_inter - m_row)
        scores = jnp.einsum('bhld,bhsd->bhls', qb, kb) * w_intra
        num = jnp.einsum('bhls,bhsv->bhlv', scores, vb) \
            + w_inter[..., None] * jnp.einsum('bhld,bhdv->bhlv', qb, c_state)
        den = jnp.sum(scores, axis=-1) + w_inter * jnp.einsum('bhld,bhd->bhl', qb, n_state)
        h_out = num / jnp.maximum(jnp.abs(den), jnp.exp(-m_row))[..., None]
        d_state = g[..., None] - b + li
        m_new = jnp.maximum(g + m_state, jnp.max(d_state, axis=-1))
        ws = jnp.exp(d_state - m_new[..., None])
        keep = jnp.exp(g + m_state - m_new)
        c_new = keep[..., None, None] * c_state + jnp.einsum('bhs,bhsd,bhsv->bhdv', ws, kb, vb)
        n_new = keep[..., None] * n_state + jnp.einsum('bhs,bhsd->bhd', ws, kb)
        return (c_new, n_new, m_new), h_out

    init = (jnp.zeros((bsz, h, dk, dv), f32), jnp.zeros((bsz, h, dk), f32), jnp.zeros((bsz, h), f32))
    _, hs = lax.scan(step, init, (qc, kc, vc, log_i, log_f))
    hs = jnp.moveaxis(hs, (0, 2), (1, 3))
    return hs.reshape(bsz, s, h, dv)


def causal_depthwise_conv(u, w):
    ch = u.shape[-1]
    return lax.conv_general_dilated(
        u, w[:, None, :].astype(u.dtype), window_strides=(1,), padding=[(CONV_K - 1, 0)],
        dimension_numbers=('NWC', 'WIO', 'NWC'), feature_group_count=ch)


def setup_inputs(seed: int = 0) -> dict:
    key = jax.random.key(seed)
    ks = jax.random.split(key, 16)
    d = D_MODEL
    nrm = jax.random.normal
    x = nrm(ks[0], (BATCH, SEQ, d), jnp.float32)
    c = nrm(ks[1], (BATCH, d), jnp.float32)
    norm1_w = 1.0 + 0.05 * nrm(ks[2], (DEPTH, d), jnp.float32)
    w_ada = 0.3 * d ** -0.5 * nrm(ks[3], (DEPTH, d, 3 * d), jnp.float32)
    b_ada = 0.02 * nrm(ks[4], (DEPTH, 3 * d), jnp.float32)
    w_in = d ** -0.5 * nrm(ks[5], (DEPTH, d, IN_WIDTH), jnp.float32)
    b_in = 0.02 * nrm(ks[6], (DEPTH, IN_WIDTH), jnp.float32)
    offs = _split_offsets()
    f_off = offs[6]
    b_in = b_in.at[:, f_off:f_off + MLSTM_HEADS].add(jnp.linspace(3.0, 6.0, MLSTM_HEADS, dtype=jnp.float32))
    conv_w = CONV_K ** -0.5 * nrm(ks[7], (DEPTH, CONV_K, CONV_WIDTH), jnp.float32)
    headnorm_w = 1.0 + 0.05 * nrm(ks[8], (DEPTH, MLSTM_V_WIDTH), jnp.float32)
    w_proj_a = MLSTM_V_WIDTH ** -0.5 * nrm(ks[9], (DEPTH, MLSTM_V_WIDTH, d), jnp.float32)
    w_proj_b = CONV_WIDTH ** -0.5 * nrm(ks[10], (DEPTH, CONV_WIDTH, d), jnp.float32)
    w_out = d ** -0.5 * nrm(ks[11], (DEPTH, d, d), jnp.float32)
    normf_w = 1.0 + 0.05 * nrm(ks[12], (d,), jnp.float32)
    return {"x": x, "c": c, "norm1_w": norm1_w, "w_ada": w_ada, "b_ada": b_ada,
            "w_in": w_in, "b_in": b_in, "conv_w": conv_w, "headnorm_w": headnorm_w,
            "w_proj_a": w_proj_a, "w_proj_b": w_proj_b, "w_out": w_out, "normf_w": normf_w}


def reference(x, c, norm1_w, w_ada, b_ada, w_in, b_in, conv_w, headnorm_w,
              w_proj_a, w_proj_b, w_out, normf_w):
    bsz, s, d = x.shape
    offs = _split_offsets()
    split_points = offs[1:]
    c_act = jax.nn.silu(c)
    for l in range(DEPTH):
        mod = c_act @ w_ada[l] + b_ada[l]
        shift, scale, gate = jnp.split(mod, 3, axis=-1)
        h = rms_norm(x, norm1_w[l]) * (1.0 + scale[:, None, :]) + shift[:, None, :]
        proj = h @ w_in[l] + b_in[l]
        (q, k, v, o, z_a, i_pre, f_pre, u, b_gate, c_gate, z_b, g_a, g_b) = \
            jnp.split(proj, split_points, axis=-1)
        h_a = mlstm_chunkwise(
            q.reshape(bsz, s, MLSTM_HEADS, MLSTM_QK_DIM),
            k.reshape(bsz, s, MLSTM_HEADS, MLSTM_QK_DIM),
            v.reshape(bsz, s, MLSTM_HEADS, MLSTM_V_DIM),
            i_pre, f_pre)
        h_a = jax.nn.sigmoid(o.astype(jnp.float32)).reshape(bsz, s, MLSTM_HEADS, MLSTM_V_DIM) * h_a
        h_a = rms_norm(h_a, headnorm_w[l].reshape(MLSTM_HEADS, MLSTM_V_DIM))
        h_a = h_a.reshape(bsz, s, MLSTM_V_WIDTH).astype(x.dtype) * jax.nn.silu(z_a)
        y_a = h_a @ w_proj_a[l]
        h_b = b_gate * causal_depthwise_conv(c_gate * u, conv_w[l])
        y_b = (h_b * jax.nn.silu(z_b)) @ w_proj_b[l]
        merged = jax.nn.sigmoid(g_a) * y_a + jax.nn.sigmoid(g_b) * y_b
        x = x + gate[:, None, :] * (merged @ w_out[l])
    return rms_norm(x, normf_w)
```

```python
import contextlib
import numpy as np
import concourse.bass as bass
import concourse.mybir as mybir
from concourse.bass_utils import run_bass_kernel_spmd

F32 = mybir.dt.float32
BF16 = mybir.dt.bfloat16
ALU = mybir.AluOpType
AF = mybir.ActivationFunctionType

D = 2048
KC = 16
S_CORE = 2048
TT = 1024
NT = TT // 128
NH = 8
EPS = 1e-6
OQ, OK_, OV, OO, OZA, OI, OF_, OU, OB, OC, OZB, OGA, OGB = (
    0, 1024, 2048, 4096, 6144, 8192, 8200, 8208, 10256, 12304, 14352, 16400, 18448)
INW = 20496
ENGS = ("sync", "scalar", "gpsimd", "vector", "tensor")


class Sem:
    def __init__(self, h):
        self.h = h
        self.n = 0


class Ev:
    __slots__ = ("sem", "val")

    def __init__(self, sem, val):
        self.sem = sem
        self.val = val


class Prog:
    def __init__(self, nc, stack):
        self.nc = nc
        self.stack = stack
        self.q = {e: [] for e in ENGS}
        self.waited = {e: {} for e in ENGS}
        self.esem = {}
        for e in ("scalar", "gpsimd", "vector", "tensor"):
            self.esem[e] = self.new_sem("es_" + e)

    def new_sem(self, name):
        return Sem(self.stack.enter_context(self.nc.semaphore(name)))

    def op(self, eng, fn, waits=(), sem=None, inc=1):
        ws = []
        for ev in _flat(waits):
            k = id(ev.sem)
            if self.waited[eng].get(k, 0) >= ev.val:
                continue
            self.waited[eng][k] = ev.val
            ws.append(ev)
        out = None
        if sem is not None:
            sem.n += inc
            out = Ev(sem, sem.n)
        self.q[eng].append((ws, fn, sem, inc))
        return out

    def dve(self, fn, waits=(), sig=True):
        return self.op("vector", fn, waits, self.esem["vector"] if sig else None)

    def act(self, fn, waits=(), sig=True):
        return self.op("scalar", fn, waits, self.esem["scalar"] if sig else None)

    def pe(self, fn, waits=(), sig=False):
        return self.op("tensor", fn, waits, self.esem["tensor"] if sig else None)

    def pool(self, fn, waits=(), sig=True):
        return self.op("gpsimd", fn, waits, self.esem["gpsimd"] if sig else None)

    def dma(self, queue, out, in_, waits, sem):
        return self.op(queue, REC.dma_start(out=out, in_=in_), waits, sem, 16)

    def emit(self, block):
        def runner(name):
            def f(e):
                for ws, fn, sem, inc in self.q[name]:
                    for w in ws:
                        e.wait_ge(w.sem.h, w.val)
                    if fn is None:
                        continue
                    ins = fn(e)
                    if sem is not None:
                        ins.then_inc(sem.h, inc)
            return f
        block.sync(runner("sync"))
        block.scalar(runner("scalar"))
        block.gpsimd(runner("gpsimd"))
        block.vector(runner("vector"))
        block.tensor(runner("tensor"))


class _Call:
    __slots__ = ("name", "args", "kwargs")

    def __init__(self, name, args, kwargs):
        self.name = name
        self.args = args
        self.kwargs = kwargs

    def __call__(self, e):
        return getattr(e, self.name)(*self.args, **self.kwargs)


class _Recorder:
    def __getattr__(self, name):
        return lambda *a, **kw: _Call(name, a, kw)


REC = _Recorder()


def _flat(x):
    if x is None:
        return
    if isinstance(x, Ev):
        yield x
        return
    for y in x:
        yield from _flat(y)


class Pool2:
    def __init__(self, bufs):
        self.bufs = bufs
        self.free = [[] for _ in bufs]
        self.i = 0

    def get(self):
        i = self.i
        self.i = (i + 1) % len(self.bufs)
        return i, self.bufs[i], self.free[i]

    def rel(self, i, *evs):
        self.free[i] = list(evs)


class _StopBuild(Exception):
    pass


def build_program(n_pre_st=2, n_main_st=2, stop=None):
    nc = bass.Bass("TRN2", target_bir_lowering=False)
    dram = lambda n, s, k="ExternalInput": nc.dram_tensor(n, s, F32, kind=k).ap()
    x_main = dram("x_main", [S_CORE, D])
    x_pre = dram("x_pre", [S_CORE, D])
    x_halo = dram("x_halo", [128, D])
    maskv = dram("maskv", [128, 1])
    c_fm = dram("c_fm", [128, KC])
    norm1_fm = dram("norm1_fm", [128, KC])
    hw_fm = dram("hw_fm", [128, KC])
    convw_fm = dram("convw_fm", [128, KC * 3])
    normf_b = dram("normf_b", [128, D])
    bif_b = dram("bif_b", [128, 16])
    ident_in = dram("ident_in", [128, 128])
    tri_in = dram("tri_in", [128, 128])
    w_ada = dram("w_ada", [D, 3 * D])
    b_ada = dram("b_ada", [1, 3 * D])
    w_in = dram("w_in", [D, INW])
    b_in = dram("b_in", [1, INW])
    w_pa = dram("w_proj_a", [D, D])
    w_pb = dram("w_proj_b", [D, D])
    w_o = dram("w_out", [D, D])
    out = dram("out", [S_CORE, D], "ExternalOutput")

    stack = contextlib.ExitStack()
    with stack:
        P = Prog(nc, stack)
        sb = lambda n, s, dt: stack.enter_context(nc.sbuf_tensor(n, s, dt))

        R1 = sb("R1", [128, 8192], F32)
        R2 = sb("R2", [128, 8192], F32)
        mbT = sb("mbT", [128, KC, TT], BF16)
        WS = [sb("ws%d" % i, [128, 17, 512], BF16) for i in range(3)]
        TR = sb("TR", [128, 6672], F32)
        Dst = sb("Dst", [128, NH, 257], F32)
        gate_b = sb("gate_b", [128, D], F32)
        normf_t = sb("normf_t", [128, D], F32)
        identb = sb("identb", [128, 128], BF16)
        maskf = sb("maskf", [128, 128], F32)
        onesf = sb("onesf", [128, 128], F32)
        onesrow = sb("onesrow", [1, 512], BF16)
        wif = sb("wif", [128, 17, 16], BF16)
        small = sb("small", [128, 512], F32)
        gates = sb("gates", [128, 6, NT * 8], F32)
        hist = sb("hist", [128, KC, 2], F32)

        hT = R1[:].bitcast(BF16).rearrange("p (a b) -> p a b", b=TT)
        hXT = R2[:].bitcast(BF16).rearrange("p (a b) -> p a b", b=TT)
        hThalo = mbT[:, 0:2, :].rearrange("p a b -> p (a b)").rearrange("p (a b) -> p a b", b=128)
        xo_lo = R1[:].rearrange("p (a b) -> p a b", b=D)
        xo_hi = R2[:].rearrange("p (a b) -> p a b", b=D)

        def TV(off_w, nwords, dt, inner=None):
            ap = TR[:, off_w:off_w + nwords]
            if dt == BF16:
                ap = ap.bitcast(BF16)
            if inner is not None:
                ap = ap.rearrange("p (a b) -> p a b", b=inner)
            return ap

        c_act = small[:, 0:16]
        n1w = small[:, 16:32]
        hw = small[:, 32:48]
        cw = small[:, 48:96].rearrange("p (a b) -> p a b", b=3)
        mod_fm = small[:, 96:128]
        A_fm = small[:, 128:144]
        maskt = small[:, 144:145]
        egc = small[:, 152:160]
        ssx = small[:, 160:176]
        rsx = small[:, 176:192]
        bifb = small[:, 192:208]
        uh = small[:, 208:212].rearrange("p (a b) -> p a b", b=2)
        rr = small[:, 224:256]
        g_if = sb("g_if", [128, NT, 16], F32)
        g_sp = gates[:, 1, :].rearrange("p (t c) -> p t c", c=8)
        g_a = gates[:, 2, :].rearrange("p (t c) -> p t c", c=8)
        g_ea = gates[:, 3, :].rearrange("p (t c) -> p t c", c=8)
        g_emb = gates[:, 4, :].rearrange("p (t c) -> p t c", c=8)
        g_eg = gates[:, 5, :].rearrange("p (t c) -> p t c", c=8)

        PS = [stack.enter_context(nc.psum_tensor("ps%d" % i, [128, 1024], F32)) for i in range(4)]
        pp = Pool2(PS)

        s_const = P.new_sem("s_const")
        s_ws = [P.new_sem("s_ws%d" % i) for i in range(3)]
        s_x = [P.new_sem("s_x%d" % i) for i in range(2)]
        s_xo = P.new_sem("s_xo")
        s_out = P.new_sem("s_out")
        wfree = [[] for _ in range(3)]
        wstate = {"i": 0}

        def wview(w):
            return w.rearrange("(kc p) n -> p kc n", p=128)

        def wload(w, c0, width, bias=None):
            i = wstate["i"]
            wstate["i"] = (i + 1) % 3
            slot = WS[i]
            ev = P.dma("gpsimd", slot[:, 0:KC, 0:width], wview(w)[:, :, c0:c0 + width], wfree[i], s_ws[i])
            if bias is not None:
                ev = P.dma("gpsimd", slot[0:1, 16, 0:width], bias[0:1, c0:c0 + width], (), s_ws[i])
            return i, slot, ev

        def wrel(i, ev):
            wfree[i] = [ev]

        cev = []
        for dst, src in ((c_act, c_fm[:, :]), (n1w, norm1_fm[:, :]), (hw, hw_fm[:, :]),
                         (small[:, 48:96], convw_fm[:, :]), (maskt, maskv[:, :]),
                         (bifb, bif_b[:, :]), (maskf[:], tri_in[:, :]), (normf_t[:], normf_b[:, :])):
            cev.append(P.dma("sync", dst, src, (), s_const))
        c_loaded_h = cev[-1]
        s_constg = P.new_sem("s_constg")
        P.dma("gpsimd", identb[:], ident_in[:, :], (), s_constg)
        c_loaded_g = P.dma("gpsimd", wif[:, 0:KC, :], wview(w_in)[:, :, OI:OI + 16], (), s_constg)
        c_loaded = [c_loaded_h, c_loaded_g]
        e_m1 = P.dve(REC.memset(onesf[:], 1.0))
        e_m2 = P.dve(REC.memset(onesrow[:], 1.0))
        e_m3 = P.dve(REC.memset(Dst[:], 0.0))
        e_m4 = P.dve(REC.memset(egc, 1.0), c_loaded)
        e_m5 = P.dve(REC.memset(hist[:], 0.0))
        e_m6 = P.dve(REC.tensor_scalar(out=normf_t[:], in0=normf_t[:], scalar1=float(D ** 0.5),
                                               scalar2=None, op0=ALU.mult), c_loaded)
        e_m7 = P.dve(REC.tensor_scalar(out=hw, in0=hw, scalar1=16.0, scalar2=None, op0=ALU.mult),
                     c_loaded)
        cst_epsD = small[:, 256:257]
        cst_eps256 = small[:, 257:258]
        cst_mhalf = small[:, 258:259]
        e_m8 = P.dve(REC.memset(cst_epsD, float(D * EPS)))
        e_m9 = P.dve(REC.memset(cst_eps256, float(256 * EPS)))
        e_m10 = P.dve(REC.memset(cst_mhalf, -0.5))
        const_ev = c_loaded + [e_m1, e_m2, e_m3, e_m4, e_m5, e_m6, e_m7, e_m8, e_m9, e_m10]

        def pool_rsqrt(dst, src, cst, waits):
            e1 = P.pool(REC.tensor_tensor(out=dst, in0=src, in1=cst, op=ALU.add), list(waits) + const_ev)
            return P.pool(REC.tensor_tensor(out=dst, in0=dst, in1=cst_mhalf, op=ALU.pow), [e1])

        sgc = rr[:, 0:16]
        e = P.act(REC.activation(out=sgc, in_=c_act, func=AF.Sigmoid), c_loaded)
        e_cact = P.dve(REC.tensor_tensor(out=c_act, in0=c_act, in1=sgc, op=ALU.mult), [e] + c_loaded)
        cact_b = TV(0, 2048, F32, 128)
        evs = []
        for kc in range(KC):
            evs.append(P.dve(REC.tensor_copy(
                out=cact_b[:, kc, :], in_=c_act[:, kc:kc + 1].to_broadcast([128, 128])), [e_cact]))
        e_cb = evs[-1]
        s_ada = [P.new_sem("s_ada0"), P.new_sem("s_ada1")]
        ada_free = [[], []]
        mod_ps_i, mod_ps, mod_free = pp.get()
        last_mod_pe = None
        ada_slots = [R2[:, 0:4096].rearrange("p (a b) -> p a b", b=256),
                     R2[:, 4096:8192].rearrange("p (a b) -> p a b", b=256)]
        ada_bias = TV(2048, 512, F32, 256)
        e_mod = None
        for blk in range(24):
            if blk == 16:
                e_mod = P.dve(REC.tensor_copy(out=mod_fm, in_=mod_ps[:, 0:32]), [last_mod_pe])
                pp.rel(mod_ps_i, e_mod)
            si = blk % 2
            slot = ada_slots[si]
            c0 = blk * 256
            ev = P.dma("sync", slot, wview(w_ada)[:, :, c0:c0 + 256], ada_free[si], s_ada[si])
            ev = P.dma("sync", ada_bias[0:1, si, :], b_ada[0:1, c0:c0 + 256], (), s_ada[si])
            if blk < 16:
                for j in range(2):
                    col = blk * 2 + j
                    for kc in range(KC):
                        P.pe(REC.matmul(
                            mod_ps[:, col:col + 1], lhsT=slot[:, kc, j * 128:(j + 1) * 128],
                            rhs=c_act[:, kc:kc + 1], start=(kc == 0), stop=False),
                            [ev, e_cact] + const_ev + mod_free if kc == 0 else ())
                    last = P.pe(REC.matmul(
                        mod_ps[:, col:col + 1], lhsT=ada_bias[0:1, si, j * 128:(j + 1) * 128],
                        rhs=onesf[0:1, 0:1], start=False, stop=True), (), sig=True)
                ada_free[si] = [last]
                last_mod_pe = last
            else:
                gi, gps, gfree = pp.get()
                gc0 = c0 - 4096
                for kc in range(KC):
                    P.pe(REC.matmul(
                        gps[:, 0:256], lhsT=cact_b[:, kc, :], rhs=slot[:, kc, :],
                        start=(kc == 0), stop=False), [ev, e_cb] + gfree if kc == 0 else ())
                last = P.pe(REC.matmul(
                    gps[:, 0:256], lhsT=onesf[0:1, :], rhs=ada_bias[0:1, si, :],
                    start=False, stop=True), (), sig=True)
                ada_free[si] = [last]
                ec = P.act(REC.activation(
                    out=gate_b[:, gc0:gc0 + 256], in_=gps[:, 0:256], func=AF.Identity), [last])
                pp.rel(gi, ec)
        e_ada_done = last
        e_A = P.dve(REC.scalar_tensor_tensor(
            out=A_fm, in0=mod_fm[:, 16:32], scalar=1.0, in1=n1w, op0=ALU.add, op1=ALU.mult), [e_mod] + c_loaded)
        e_A = P.dve(REC.tensor_scalar(out=A_fm, in0=A_fm, scalar1=float(D ** 0.5), scalar2=None,
                                              op0=ALU.mult), [e_A])
        shift_fm = mod_fm[:, 0:16]
        mod_ready = [e_A, e_mod]

        st = {"R1_free": [], "R2_free": [e_ada_done], "TR_free": [e_cb, e_ada_done],
              "mbT_free": [], "out_evs": []}

        def stage_A(xsrc, ntiles, dst, waits_dst, tr_waits):
            xt = [TV(0, 2048, F32), TV(2048, 2048, F32)]
            xn = [TV(4096, 1024, BF16), TV(5120, 1024, BF16)]
            xfree = [list(tr_waits), list(tr_waits)]
            nfree = [list(tr_waits), list(tr_waits)]
            done = []
            for t in range(ntiles):
                bi = t % 2
                e_ld = P.dma("sync", xt[bi], xsrc[t * 128:(t + 1) * 128, :], xfree[bi], s_x[bi])
                ssc = ssx[:, bi:bi + 1]
                rsc = rsx[:, bi:bi + 1]
                e_sq = P.act(REC.activation(
                    out=xn[bi], in_=xt[bi], func=AF.Square, accum_out=ssc), [e_ld] + nfree[bi])
                e_r2 = pool_rsqrt(rsc, ssc, cst_epsD, [e_sq] + xfree[bi])
                e_xn = P.dve(REC.tensor_scalar(
                    out=xn[bi], in0=xt[bi], scalar1=rsc, scalar2=None, op0=ALU.mult), [e_r2, e_sq, e_ld])
                xfree[bi] = [e_xn]
                pi, ps, pfree = pp.get()
                psb = ps[:].bitcast(BF16).rearrange("p (a b) -> p a b", b=128)
                for c in range(KC):
                    e_tr = P.pe(REC.transpose(
                        out=psb[:, c, :], in_=xn[bi][:, c * 128:(c + 1) * 128], identity=identb[:]),
                        [e_xn] + pfree + const_ev if c == 0 else (), sig=(c == KC - 1))
                nfree[bi] = [e_tr]
                evs = []
                for c in range(KC):
                    o = dst[:, c, t * 128:(t + 1) * 128]
                    if c < KC // 2:
                        evs.append(P.act(REC.activation(
                            out=o, in_=psb[:, c, :], func=AF.Identity,
                            scale=A_fm[:, c:c + 1], bias=shift_fm[:, c:c + 1]),
                            [e_tr] + mod_ready + waits_dst, sig=(c in (KC // 2 - 1, KC - 1))))
                    else:
                        evs.append(P.dve(REC.tensor_scalar(
                            out=o, in0=psb[:, c, :], scalar1=A_fm[:, c:c + 1], scalar2=shift_fm[:, c:c + 1],
                            op0=ALU.mult, op1=ALU.add), [e_tr] + mod_ready + waits_dst, sig=(c in (KC // 2 - 1, KC - 1))))
                pp.rel(pi, evs[KC - 1], evs[KC // 2 - 1])
                done += [evs[KC - 1], evs[KC // 2 - 1]]
            return done, xfree[0] + xfree[1] + nfree[0] + nfree[1]

        def stage_B(hT_ready, first):
            e_c = None
            if not first:
                e_c = P.dve(REC.tensor_copy(out=egc, in_=g_eg[:, NT - 1, :]), st["gates_last"])
            pi, ps, pfree = pp.get()
            psg = ps[:, 0:NT * 16].rearrange("p (t c) -> p t c", c=16)
            for t in range(NT):
                for kc in range(KC):
                    last = P.pe(REC.matmul(
                        psg[:, t, :], lhsT=hT[:, kc, t * 128:(t + 1) * 128], rhs=wif[:, kc, :],
                        start=(kc == 0), stop=(kc == KC - 1)),
                        hT_ready + pfree + const_ev if (t == 0 and kc == 0) else (),
                        sig=(t == NT - 1 and kc == KC - 1))
            wg = st.get("gates_last", []) + ([e_c] if e_c is not None else [])
            for t in range(NT):
                e1 = P.dve(REC.tensor_tensor(
                    out=g_if[:, t, :], in0=psg[:, t, :], in1=bifb, op=ALU.add), [last] + wg + const_ev)
            e2 = P.act(REC.activation(out=g_sp, in_=g_if[:, :, 8:16], func=AF.Exp, scale=-1.0), [e1] + wg)
            e3 = P.act(REC.activation(out=g_sp, in_=g_sp, func=AF.Ln, bias=1.0), [e2])
            psb_ = ps[:, 512:512 + NT * 8].rearrange("p (t c) -> p t c", c=8)
            psg_ = ps[:, 768:768 + NT * 8].rearrange("p (t c) -> p t c", c=8)
            for t in range(NT):
                P.pe(REC.matmul(psb_[:, t, :], lhsT=maskf[:], rhs=g_sp[:, t, :],
                                             start=True, stop=True), [e3, e1])
                e4 = P.pe(REC.matmul(psg_[:, t, :], lhsT=onesf[:], rhs=g_sp[:, t, :],
                                                  start=True, stop=True), (), sig=(t == NT - 1))
            e5 = P.dve(REC.tensor_tensor(out=g_a, in0=psb_, in1=g_if[:, :, 0:8], op=ALU.add), [e4, e1] + wg)
            e6 = P.act(REC.activation(out=g_ea, in_=g_a, func=AF.Exp), [e5] + wg)
            e7 = P.act(REC.activation(out=g_emb, in_=psb_, func=AF.Exp), [e4, e5] + wg)
            e8 = P.act(REC.activation(out=g_eg, in_=psg_, func=AF.Exp, scale=-1.0), [e4, e5] + wg)
            pp.rel(pi, e5, e8)
            return [e6, e7, e8]

        def fm_group(ps_ap, slot, c_in_slot, act_ap, ntok, first_waits, bias=True):
            last = None
            for h0 in range(0, ntok, 512):
                n = min(512, ntok - h0)
                for kc in range(KC):
                    last = P.pe(REC.matmul(
                        ps_ap[:, h0:h0 + n], lhsT=slot[:, kc, c_in_slot:c_in_slot + 128],
                        rhs=act_ap[:, kc, h0:h0 + n], start=(kc == 0), stop=(not bias and kc == KC - 1)),
                        first_waits if (kc == 0 and h0 == 0) else (),
                        sig=(not bias and kc == KC - 1 and h0 + n >= ntok))
                if bias:
                    last = P.pe(REC.matmul(
                        ps_ap[:, h0:h0 + n], lhsT=slot[0:1, 16, c_in_slot:c_in_slot + 128],
                        rhs=onesrow[0:1, 0:n], start=False, stop=True), (), sig=(h0 + n >= ntok))
            return last

        def tm_group(ps_ap, slot, width, act_tile, first_waits, bias=True):
            last = None
            for kc in range(KC):
                last = P.pe(REC.matmul(
                    ps_ap, lhsT=act_tile(kc), rhs=slot[:, kc, 0:width],
                    start=(kc == 0), stop=(not bias and kc == KC - 1)),
                    first_waits if kc == 0 else (), sig=(not bias and kc == KC - 1))
            if bias:
                last = P.pe(REC.matmul(
                    ps_ap, lhsT=onesrow[0:1, 0:128], rhs=slot[0:1, 16, 0:width],
                    start=False, stop=True), (), sig=True)
            return last

        def prefix_state(hT_ready, gate_ev):
            ktok = TV(0, 2048, BF16, 512)
            vp = TV(2048, 4 * NT * 129, BF16)
            vp = vp.rearrange("p (t h c) -> p t h c", h=4, c=258)
            tr_last = []
            for g in range(2):
                wi, slot, wev = wload(w_in, OK_ + g * 512, 512, b_in)
                evk = []
                for t2 in range(NT // 2):
                    pi, ps, pfree = pp.get()
                    for j in range(2):
                        t = t2 * 2 + j
                        last = tm_group(ps[:, j * 512:(j + 1) * 512], slot, 512,
                                        lambda kc, t=t: hT[:, kc, t * 128:(t + 1) * 128],
                                        [wev] + hT_ready + pfree + const_ev + st["TR_free"] + tr_last)
                    ek = P.act(REC.activation(
                        out=ktok[:, 2 * t2:2 * t2 + 2, :], in_=ps[:].rearrange("p (a b) -> p a b", b=512),
                        func=AF.Identity), [last] + st["TR_free"] + tr_last)
                    pp.rel(pi, ek)
                    evk.append(ek)
                wrel(wi, last)
                chk('pk')
                evv = []
                for j2 in range(2):
                    wi, slot, wev = wload(w_in, OV + g * 1024 + j2 * 512, 512, b_in)
                    for t in range(NT):
                        pi, ps, pfree = pp.get()
                        last = tm_group(ps[:, 0:512], slot, 512,
                                        lambda kc, t=t: hT[:, kc, t * 128:(t + 1) * 128],
                                        [wev] + hT_ready + pfree)
                        es = []
                        for hd in range(2):
                            hh = j2 * 2 + hd
                            head = g * 4 + hh
                            es.append(P.dve(REC.tensor_scalar(
                                out=vp[:, t, hh, 0:256], in0=ps[:, hd * 256:(hd + 1) * 256],
                                scalar1=g_ea[:, t, head:head + 1], scalar2=None, op0=ALU.mult),
                                [last] + gate_ev + st["TR_free"] + tr_last))
                        pp.rel(pi, *es)
                        evv += es
                    wrel(wi, last)
                for hh in range(4):
                    head = g * 4 + hh
                    evv.append(P.dve(REC.tensor_copy(
                        out=vp[:, :, hh, 256], in_=g_ea[:, :, head]), gate_ev + st["TR_free"] + tr_last))
                chk('pv')
                tr_last = []
                for t in range(NT):
                    for hh in range(4):
                        head = g * 4 + hh
                        pi, ps, pfree = pp.get()
                        em = P.pe(REC.matmul(
                            ps[:, 0:257], lhsT=ktok[:, t, hh * 128:(hh + 1) * 128], rhs=vp[:, t, hh, 0:257],
                            start=True, stop=True), evk + evv + pfree, sig=True)
                        sc = egc[:, head:head + 1] if t == 0 else g_eg[:, t - 1, head:head + 1]
                        ed = P.dve(REC.scalar_tensor_tensor(
                            out=Dst[:, head, :], in0=Dst[:, head, :], scalar=sc, in1=ps[:, 0:257],
                            op0=ALU.mult, op1=ALU.add), [em] + gate_ev + st["D_ev"][head])
                        st["D_ev"][head] = [ed]
                        pp.rel(pi, ed)
                        tr_last = [em]
            st["TR_free"] = tr_last
            st["hT_readers"] = tr_last

        def branch_B(hT_ready, first_main):
            cu = TV(0, 2 * 1026, F32, 1026)
            yy = TV(2052, 2 * 1024, F32, 1024)
            stmp = TV(4100, 2 * 1024, F32, 1024)
            trf = st["TR_free"]
            rd_ev = []
            last_pe = None
            for cbp in range(8):
                wi, slot, wev = wload(w_in, OU + cbp * 256, 256, b_in)
                e_u = []
                e_uh = []
                for j in range(2):
                    pi, ps, pfree = pp.get()
                    last = fm_group(ps, slot, j * 128, hT, TT, [wev] + hT_ready + pfree + const_ev)
                    if first_main:
                        pass
                    eu = P.act(REC.activation(out=cu[:, j, 2:1026], in_=ps[:, :], func=AF.Identity),
                               [last] + trf + rd_ev)
                    pp.rel(pi, eu)
                    e_u.append(eu)
                if first_main:
                    pi, ps, pfree = pp.get()
                    for j in range(2):
                        for kc in range(KC):
                            P.pe(REC.matmul(
                                ps[:, 2 * j:2 * j + 2], lhsT=slot[:, kc, j * 128:(j + 1) * 128],
                                rhs=hThalo[:, kc, 126:128], start=(kc == 0), stop=False),
                                st["halo_ready"] + pfree if (kc == 0 and j == 0) else ())
                        last = P.pe(REC.matmul(
                            ps[:, 2 * j:2 * j + 2], lhsT=slot[0:1, 16, j * 128:(j + 1) * 128],
                            rhs=onesrow[0:1, 0:2], start=False, stop=True), (), sig=True)
                    euh = P.dve(REC.tensor_copy(
                        out=uh, in_=ps[:, 0:4].rearrange("p (a b) -> p a b", b=2)), [last] + rd_ev)
                    pp.rel(pi, euh)
                    e_uh = [euh]
                wrel(wi, last)
                wi, slot, wev = wload(w_in, OC + cbp * 256, 256, b_in)
                e_cu = []
                for j in range(2):
                    cb = cbp * 2 + j
                    pi, ps, pfree = pp.get()
                    last = fm_group(ps, slot, j * 128, hT, TT, [wev] + pfree)
                    ec = P.dve(REC.tensor_tensor(
                        out=cu[:, j, 2:1026], in0=ps[:, :], in1=cu[:, j, 2:1026], op=ALU.mult), [last, e_u[j]])
                    pp.rel(pi, ec)
                    if not first_main:
                        eh = P.dve(REC.tensor_copy(out=cu[:, j, 0:2], in_=hist[:, cb, :]),
                                   rd_ev + trf + st["hist_ev"])
                        e_cu.append([ec, eh])
                    else:
                        e_cu.append([ec])
                if first_main:
                    pi, ps, pfree = pp.get()
                    for j in range(2):
                        for kc in range(KC):
                            P.pe(REC.matmul(
                                ps[:, 2 * j:2 * j + 2], lhsT=slot[:, kc, j * 128:(j + 1) * 128],
                                rhs=hThalo[:, kc, 126:128], start=(kc == 0), stop=False),
                                pfree if (kc == 0 and j == 0) else ())
                        last = P.pe(REC.matmul(
                            ps[:, 2 * j:2 * j + 2], lhsT=slot[0:1, 16, j * 128:(j + 1) * 128],
                            rhs=onesrow[0:1, 0:2], start=False, stop=True), (), sig=True)
                    eh1 = P.dve(REC.scalar_tensor_tensor(
                        out=cu[:, :, 0:2], in0=ps[:, 0:4].rearrange("p (a b) -> p a b", b=2),
                        scalar=maskt, in1=uh, op0=ALU.mult, op1=ALU.mult), [last] + e_uh + rd_ev + trf + const_ev)
                    pp.rel(pi, eh1)
                    for j in range(2):
                        e_cu[j].append(eh1)
                wrel(wi, last)
                e_y = []
                e_hist = []
                for j in range(2):
                    cb = cbp * 2 + j
                    y1 = P.dve(REC.tensor_scalar(
                        out=yy[:, j, :], in0=cu[:, j, 2:1026], scalar1=cw[:, cb, 2:3], scalar2=None,
                        op0=ALU.mult), e_cu[j] + rd_ev + const_ev)
                    y2 = P.dve(REC.scalar_tensor_tensor(
                        out=yy[:, j, :], in0=cu[:, j, 1:1025], scalar=cw[:, cb, 1:2], in1=yy[:, j, :],
                        op0=ALU.mult, op1=ALU.add), [y1])
                    y3 = P.dve(REC.scalar_tensor_tensor(
                        out=yy[:, j, :], in0=cu[:, j, 0:1024], scalar=cw[:, cb, 0:1], in1=yy[:, j, :],
                        op0=ALU.mult, op1=ALU.add), [y2])
                    eh = P.dve(REC.tensor_copy(out=hist[:, cb, :], in_=cu[:, j, 1024:1026]),
                               e_cu[j] + st["hist_ev"])
                    e_y.append(y3)
                    e_hist.append(eh)
                wi, slot, wev = wload(w_in, OB + cbp * 256, 256, b_in)
                e_b = []
                for j in range(2):
                    pi, ps, pfree = pp.get()
                    last = fm_group(ps, slot, j * 128, hT, TT, [wev] + pfree)
                    eb = P.dve(REC.tensor_tensor(
                        out=yy[:, j, :], in0=ps[:, :], in1=yy[:, j, :], op=ALU.mult), [last, e_y[j]])
                    pp.rel(pi, eb)
                    e_b.append(eb)
                wrel(wi, last)
                wi, slot, wev = wload(w_in, OZB + cbp * 256, 256, b_in)
                new_rd = []
                for j in range(2):
                    cb = cbp * 2 + j
                    pi, ps, pfree = pp.get()
                    last = fm_group(ps, slot, j * 128, hT, TT, [wev] + pfree)
                    es = P.act(REC.activation(out=stmp[:, j, :], in_=ps[:, :], func=AF.Sigmoid),
                               [last] + rd_ev + trf)
                    e1 = P.dve(REC.tensor_tensor(
                        out=yy[:, j, :], in0=yy[:, j, :], in1=stmp[:, j, :], op=ALU.mult), [es, e_b[j]])
                    e2 = P.dve(REC.tensor_tensor(
                        out=hXT[:, cb, :], in0=ps[:, :], in1=yy[:, j, :], op=ALU.mult),
                        [e1, last] + st["R2_free"])
                    pp.rel(pi, e2, es)
                    new_rd += [e2]
                wrel(wi, last)
                last_pe = last
                rd_ev = new_rd + e_hist
                st["hist_ev"] = e_hist
            st["TR_free"] = rd_ev
            return rd_ev

        def merge_stage(w_proj, og, actT, act_ready, hT_ready, accumulate):
            stmp = TV(4100, 2 * 1024, F32, 1024)
            trf = st["TR_free"]
            rd = [[], []]
            outs = []
            last = None
            for db in range(4):
                wi, slotP, wevP = wload(w_proj, db * 512, 512, None)
                wi2, slotG, wevG = wload(w_in, og + db * 512, 512, b_in)
                for j in range(4):
                    blk = db * 4 + j
                    k = blk % 2
                    pi, psG, pfree = pp.get()
                    lastG = fm_group(psG, slotG, j * 128, hT, TT, [wevG] + hT_ready + pfree + const_ev)
                    es = P.act(REC.activation(out=stmp[:, k, :], in_=psG[:, :], func=AF.Sigmoid),
                               [lastG] + rd[k] + trf)
                    pp.rel(pi, es)
                    pi, psY, pfree = pp.get()
                    last = fm_group(psY, slotP, j * 128, actT, TT, [wevP] + act_ready + pfree, bias=False)
                    if not accumulate:
                        eo = P.dve(REC.tensor_tensor(
                            out=mbT[:, blk, :], in0=psY[:, :], in1=stmp[:, k, :], op=ALU.mult),
                            [last, es] + st["mbT_free"])
                        pp.rel(pi, eo)
                    else:
                        e1 = P.dve(REC.tensor_tensor(
                            out=stmp[:, k, :], in0=psY[:, :], in1=stmp[:, k, :], op=ALU.mult), [last, es])
                        pp.rel(pi, e1)
                        eo = P.dve(REC.tensor_tensor(
                            out=mbT[:, blk, :], in0=stmp[:, k, :], in1=mbT[:, blk, :], op=ALU.add), [e1])
                    rd[k] = [eo]
                    outs.append(eo)
                wrel(wi, last)
                wrel(wi2, lastG)
            st["TR_free"] = rd[0] + rd[1]
            return outs, [last, lastG]

        def heads_stage(hT_ready, gate_ev):
            qT = TV(0, 512, BF16)
            kT = TV(512, 512, BF16)
            ktok = TV(1024, 512, BF16, 128)
            vp = TV(1536, NT * 129, BF16, 258)
            sgo = TV(2568, 1024, BF16, 256)
            zsl = TV(3592, 1024, BF16, 1024)
            zsg = TV(4616, 1024, F32)
            PT = [TV(5640, 64, BF16), TV(5704, 64, BF16)]
            Sbf = [TV(5768, 130, BF16), TV(5898, 130, BF16)]
            hs = [TV(6028, 257, F32), TV(6285, 257, F32)]
            hn = [TV(6542, 128, BF16), TV(4616 + 200, 128, BF16)]
            sqjunk = TV(4616, 129, BF16)[:, 0:257]
            trf = st["TR_free"]
            e_hc = [P.dve(REC.memset(hs[i][:, 256:257], float((256 * EPS) ** 0.5)), trf) for i in range(2)]
            prev = list(trf) + e_hc
            R2w = st["R2_free"]
            fin = []
            for h in range(NH):
                if h == 0:
                    qk_pref = [wload(w_in, OQ, 128, b_in), wload(w_in, OK_, 128, b_in)]
                wi, slot, wev = qk_pref[0]
                pi, ps, pfree = pp.get()
                last = fm_group(ps, slot, 0, hT, TT, [wev] + hT_ready + pfree + const_ev)
                e_q = P.act(REC.activation(out=qT, in_=ps[:, :], func=AF.Identity,
                                                          scale=float(128 ** -0.5)), [last] + prev)
                pp.rel(pi, e_q)
                wrel(wi, last)
                wi, slot, wev = qk_pref[1]
                pi, ps, pfree = pp.get()
                last = fm_group(ps, slot, 0, hT, TT, [wev] + pfree)
                e_k = P.dve(REC.tensor_copy(out=kT, in_=ps[:, :]), [last] + prev)
                pp.rel(pi, e_k)
                wrel(wi, last)
                pi, ps, pfree = pp.get()
                psb = ps[:, 0:512].bitcast(BF16).rearrange("p (a b) -> p a b", b=128)
                for t in range(NT):
                    e_tr = P.pe(REC.transpose(
                        out=psb[:, t, :], in_=kT[:, t * 128:(t + 1) * 128], identity=identb[:]),
                        [e_k] + pfree if t == 0 else (), sig=(t == NT - 1))
                e_kt = P.act(REC.activation(out=ktok, in_=psb, func=AF.Identity), [e_tr] + prev)
                pp.rel(pi, e_kt)
                wi, slot, wev = wload(w_in, OV + h * 256, 256, b_in)
                e_v = []
                for t4 in range(NT // 4):
                    pi, ps, pfree = pp.get()
                    for j in range(4):
                        t = t4 * 4 + j
                        last = tm_group(ps[:, j * 256:(j + 1) * 256], slot, 256,
                                        lambda kc, t=t: hT[:, kc, t * 128:(t + 1) * 128], [wev] + pfree)
                    es = []
                    for j in range(4):
                        t = t4 * 4 + j
                        fn = REC.tensor_scalar(
                            out=vp[:, t, 0:256], in0=ps[:, j * 256:(j + 1) * 256],
                            scalar1=g_ea[:, t, h:h + 1], scalar2=None, op0=ALU.mult)
                        es.append(P.dve(fn, [last] + gate_ev + prev))
                    pp.rel(pi, *es)
                    e_v += es
                wrel(wi, last)
                e_v.append(P.dve(REC.tensor_copy(out=vp[:, :, 256], in_=g_ea[:, :, h]), gate_ev + prev))
                wi, slot, wev = wload(w_in, OO + h * 256, 256, b_in)
                e_o = []
                for t4 in range(NT // 4):
                    pi, ps, pfree = pp.get()
                    for j in range(4):
                        t = t4 * 4 + j
                        last = tm_group(ps[:, j * 256:(j + 1) * 256], slot, 256,
                                        lambda kc, t=t: hT[:, kc, t * 128:(t + 1) * 128], [wev] + pfree)
                    eo = P.act(REC.activation(
                        out=sgo[:, t4 * 4:(t4 + 1) * 4, :], in_=ps[:].rearrange("p (a b) -> p a b", b=256),
                        func=AF.Sigmoid), [last] + prev)
                    pp.rel(pi, eo)
                    e_o.append(eo)
                wrel(wi, last)
                wi, slot, wev = wload(w_in, OZA + h * 256, 256, b_in)
                e_z = []
                ezprev = list(prev)
                for j in range(2):
                    pi, ps, pfree = pp.get()
                    last = fm_group(ps, slot, j * 128, hT, TT, [wev] + pfree)
                    es = P.act(REC.activation(out=zsg, in_=ps[:, :], func=AF.Sigmoid), [last] + ezprev)
                    ez = P.dve(REC.tensor_tensor(
                        out=zsl[:, j, :], in0=ps[:, :], in1=zsg, op=ALU.mult), [es, last] + prev)
                    pp.rel(pi, ez)
                    ezprev = [ez]
                    e_z.append(ez)
                wrel(wi, last)
                if h + 1 < NH:
                    qk_pref = [wload(w_in, OQ + (h + 1) * 128, 128, b_in),
                               wload(w_in, OK_ + (h + 1) * 128, 128, b_in)]
                e_s = P.act(REC.activation(out=Sbf[0][:, 0:257], in_=Dst[:, h, :], func=AF.Identity,
                                                   scale=egc[:, h:h + 1]), st["D_ev"][h] + gate_ev + prev + const_ev)
                sbf_ev = [e_s]
                pt_free = [list(prev), list(prev)]
                sbf_free = [list(prev), list(prev)]
                hs_free = [list(prev), list(prev)]
                hn_free = [list(prev), list(prev)]
                pend = None
                head_done = []

                def finish(pd):
                    (t, k, piB, psB, e_num, e_hsq, hn_w) = pd
                    psT = psB[:, 260:388].bitcast(BF16).rearrange("p (a b) -> p a b", b=128)
                    for j in range(2):
                        e_tr = P.pe(REC.transpose(
                            out=psT[:, j, :], in_=hn[k][:, j * 128:(j + 1) * 128], identity=identb[:]),
                            [hn_w] if j == 0 else (), sig=(j == 1))
                    evs = []
                    for j in range(2):
                        ch = 2 * h + j
                        evs.append(P.dve(REC.scalar_tensor_tensor(
                            out=hXT[:, ch, t * 128:(t + 1) * 128], in0=psT[:, j, :], scalar=hw[:, ch:ch + 1],
                            in1=zsl[:, j, t * 128:(t + 1) * 128], op0=ALU.mult, op1=ALU.mult),
                            [e_tr] + e_z + R2w + const_ev))
                    pp.rel(piB, *evs)
                    return [e_tr], evs

                for t in range(NT):
                    k = t % 2
                    piA, psA, pfreeA = pp.get()
                    qs = qT[:, t * 128:(t + 1) * 128]
                    ks = kT[:, t * 128:(t + 1) * 128]
                    e_sc = P.pe(REC.matmul(
                        psA[:, 0:128], lhsT=ks, rhs=qs, start=True, stop=True),
                        [e_q, e_k] + pfreeA, sig=True)
                    e_dc = P.pe(REC.matmul(
                        psA[:, 512:769], lhsT=ktok[:, t, :], rhs=vp[:, t, 0:257], start=True, stop=True),
                        [e_kt] + e_v, sig=True)
                    e_pt = P.dve(REC.tensor_tensor(
                        out=PT[k], in0=psA[:, 0:128], in1=maskf[:], op=ALU.mult), [e_sc] + pt_free[k] + const_ev)
                    sc = egc[:, h:h + 1] if t == 0 else g_eg[:, t - 1, h:h + 1]
                    e_d = P.dve(REC.scalar_tensor_tensor(
                        out=Dst[:, h, :], in0=Dst[:, h, :], scalar=sc, in1=psA[:, 512:769],
                        op0=ALU.mult, op1=ALU.add), [e_dc] + st["D_ev"][h] + sbf_ev + gate_ev)
                    st["D_ev"][h] = [e_d]
                    pp.rel(piA, e_pt, e_d)
                    piB, psB, pfreeB = pp.get()
                    P.pe(REC.matmul(
                        psB[:, 0:257], lhsT=PT[k], rhs=vp[:, t, 0:257], start=True, stop=False),
                        [e_pt] + pfreeB)
                    e_num = P.pe(REC.matmul(
                        psB[:, 0:257], lhsT=qs, rhs=Sbf[k][:, 0:257], start=False, stop=True),
                        sbf_ev, sig=True)
                    pt_free[k] = [e_num]
                    sbf_free[k] = [e_num]
                    if t + 1 < NT:
                        e_s = P.act(REC.activation(
                            out=Sbf[1 - k][:, 0:257], in_=Dst[:, h, :], func=AF.Identity,
                            scale=g_eg[:, t, h:h + 1]), [e_d] + sbf_free[1 - k] + gate_ev)
                        sbf_ev = [e_s]
                    if pend is not None:
                        trs, evs = finish(pend)
                        hn_free[pend[1]] = trs
                        head_done = evs
                    rc = rr[:, (h % 2) * 16 + 2 * t:(h % 2) * 16 + 2 * t + 1]
                    sc2 = rr[:, (h % 2) * 16 + 2 * t + 1:(h % 2) * 16 + 2 * t + 2]
                    e_r0 = P.act(REC.activation(
                        out=rc, in_=psB[:, 256:257], func=AF.Abs), [e_num])
                    e_r = P.dve(REC.tensor_scalar(
                        out=rc, in0=rc, scalar1=g_emb[:, t, h:h + 1], scalar2=None, op0=ALU.max),
                        [e_r0] + gate_ev)
                    e_r = P.dve(REC.reciprocal(out=rc, in_=rc), [e_r])
                    e_hs = P.dve(REC.scalar_tensor_tensor(
                        out=hs[k][:, 0:256], in0=psB[:, 0:256], scalar=rc, in1=sgo[:, t, :],
                        op0=ALU.mult, op1=ALU.mult), [e_r] + e_o + hs_free[k])
                    e_sq = P.act(REC.activation(
                        out=sqjunk, in_=hs[k], func=AF.Square, accum_out=sc2), [e_hs] + e_z)
                    e_r2 = P.pool(REC.tensor_tensor(out=sc2, in0=sc2, in1=cst_mhalf, op=ALU.pow),
                                  [e_sq] + const_ev)
                    e_hn = P.act(REC.activation(
                        out=hn[k], in_=hs[k][:, 0:256], func=AF.Identity, scale=sc2),
                        [e_r2, e_sq] + hn_free[k] + e_z)
                    hs_free[k] = [e_hn]
                    pend = (t, k, piB, psB, e_num, e_sq, e_hn)
                trs, evs = finish(pend)
                head_done = evs
                prev = evs + trs + [e_hn, e_num, e_d]
                fin = evs
            st["TR_free"] = prev
            return fin, prev

        def final_stage(tok0, mb_ready, dead_ev):
            tmp = [TV(0, 512, F32), TV(512, 512, F32)]
            trf = st["TR_free"]
            xe = []
            for t in range(NT):
                dst = (xo_lo if t < 4 else xo_hi)[:, t % 4, :]
                xe.append(P.dma("sync", dst, x_main[tok0 + t * 128:tok0 + (t + 1) * 128, :], dead_ev, s_xo))
            x_all = xe[-1]
            tfree = [list(trf), list(trf)]
            acc_ev = [[] for _ in range(NT)]
            last = None
            for db in range(4):
                wi, slot, wev = wload(w_o, db * 512, 512, None)
                for t in range(NT):
                    xo_t = (xo_lo if t < 4 else xo_hi)[:, t % 4, db * 512:(db + 1) * 512]
                    k = t % 2
                    pi, ps, pfree = pp.get()
                    last = tm_group(ps[:, 0:512], slot, 512,
                                    lambda kc, t=t: mbT[:, kc, t * 128:(t + 1) * 128],
                                    [wev] + mb_ready + pfree + const_ev, bias=False)
                    e1 = P.dve(REC.tensor_tensor(
                        out=tmp[k], in0=ps[:, 0:512], in1=gate_b[:, db * 512:(db + 1) * 512], op=ALU.mult),
                        [last] + tfree[k])
                    pp.rel(pi, e1)
                    e2 = P.dve(REC.tensor_tensor(
                        out=xo_t, in0=tmp[k], in1=xo_t, op=ALU.add), [e1, x_all])
                    tfree[k] = [e2]
                    acc_ev[t] = [e2]
                wrel(wi, last)
            st["mbT_free"] = [last]
            chk('finA')
            outs = []
            for t in range(NT):
                xo_t = (xo_lo if t < 4 else xo_hi)[:, t % 4, :]
                ssc = ssx[:, 2 + (t % 2):3 + (t % 2)]
                junk = TV(1024, 1024, BF16)
                e_sq = P.act(REC.activation(
                    out=junk, in_=xo_t, func=AF.Square, accum_out=ssc), acc_ev[t] + outs[-2:])
                e_r2 = pool_rsqrt(ssc, ssc, cst_epsD, [e_sq])
                e_o = P.dve(REC.scalar_tensor_tensor(
                    out=xo_t, in0=xo_t, scalar=ssc, in1=normf_t[:], op0=ALU.mult, op1=ALU.mult),
                    [e_r2, e_sq] + const_ev)
                e_st = P.dma("sync", out[tok0 + t * 128:tok0 + (t + 1) * 128, :], xo_t, [e_o], s_out)
                outs.append(e_o)
                st["out_last"] = e_st
            st["TR_free"] = tfree[0] + tfree[1]
            st["R1_free"] = [st["out_last"]]
            st["R2_free"] = [st["out_last"]]

        def chk(tag):
            if stop == tag:
                raise _StopBuild()

        def main_emit():
            st["D_ev"] = [[e_m3] for _ in range(NH)]
            st["hist_ev"] = [e_m5]
            st["gates_last"] = []
            first = True
            for s_i in range(n_pre_st):
                hev, trl = stage_A(x_pre[s_i * TT:(s_i + 1) * TT, :], NT, hT,
                                   st["R1_free"] + st.get("hT_readers", []), st["TR_free"])
                st["TR_free"] = trl
                gev = stage_B(hev, first)
                chk('preB')
                first = False
                prefix_state(hev, gev)
                chk('prefix1')
                st["gates_last"] = st["TR_free"] + [x for h in range(NH) for x in st["D_ev"][h]]
            if n_pre_st > 0:
                for h in range(NH):
                    ed = P.dve(REC.tensor_scalar(
                        out=Dst[:, h, :], in0=Dst[:, h, :], scalar1=g_eg[:, NT - 1, h:h + 1], scalar2=maskt,
                        op0=ALU.mult, op1=ALU.mult), st["D_ev"][h] + const_ev)
                    st["D_ev"][h] = [ed]
                st["gates_last"] = st["gates_last"] + [ed]
            hev, trl = stage_A(x_halo, 1, hThalo, [], st["TR_free"])
            st["TR_free"] = trl
            st["halo_ready"] = hev
            chk('halo')
            for s_i in range(n_main_st):
                tok0 = s_i * TT
                hev, trl = stage_A(x_main[tok0:tok0 + TT, :], NT, hT,
                                   st["R1_free"] + st.get("hT_readers", []), st["TR_free"])
                st["TR_free"] = trl
                chk('A')
                gev = stage_B(hev, first and s_i == 0)
                chk('B')
                if s_i == 0 and n_pre_st > 0:
                    e_fix = P.dve(REC.memset(egc, 1.0), gev + st["gates_last"])
                    gev = gev + [e_fix]
                hb_ev = branch_B(hev, s_i == 0)
                chk('brB')
                mb_ev, pe_last = merge_stage(w_pb, OGB, hXT, hb_ev, hev, False)
                st["R2_free"] = st["R2_free"] + [pe_last[0]]
                chk('mergeB')
                ha_ev, tr_ev = heads_stage(hev, gev)
                chk('heads')
                st["gates_last"] = tr_ev + [x for h in range(NH) for x in st["D_ev"][h]]
                mb_ev2, pe_last2 = merge_stage(w_pa, OGA, hXT, ha_ev, hev, True)
                chk('mergeA')
                final_stage(tok0, mb_ev2, pe_last2 + mb_ev2)
                st["hT_readers"] = []
        try:
            chk('ada')
            main_emit()
            P.op("sync", None, [st["out_last"]])
        except _StopBuild:
            lasts = [Ev(P.esem[e], P.esem[e].n) for e in ("scalar", "gpsimd", "vector", "tensor") if P.esem[e].n > 0]
            lasts += [Ev(sm, sm.n) for sm in s_ws if sm.n > 0]
            ed = P.dma("sync", out[0:128, :], gate_b[:], lasts, s_out)
            mbw = mbT[:].rearrange("p a b -> p (a b)").bitcast(F32)
            for r4 in range(4):
                ed = P.dma("sync", out[128 * (r4 + 1):128 * (r4 + 2), :], mbw[:, r4 * 2048:(r4 + 1) * 2048], lasts, s_out)
            ed = P.dma("sync", out[640:768, :], Dst[:].rearrange("p a b -> p (a b)")[:, 0:2048], lasts, s_out)
            ed = P.dma("sync", out[768:896, 0:384], gates[:].rearrange("p a b -> p (a b)"), lasts, s_out)
            for r4 in range(4):
                ed = P.dma("sync", out[896 + 128 * r4:1024 + 128 * r4, :], R1[:, r4 * 2048:(r4 + 1) * 2048], lasts, s_out)
            P.op("sync", None, [ed])

        with nc.Block() as block:
            P.emit(block)
    return nc


def _prep_inputs(x, c, norm1_w, w_ada, b_ada, w_in, b_in, conv_w, headnorm_w,
                 w_proj_a, w_proj_b, w_out, normf_w):
    f = lambda a: np.ascontiguousarray(np.asarray(a, dtype=np.float32))
    x = f(x)
    fm = lambda v: f(np.asarray(v, dtype=np.float32).reshape(KC, 128).T)
    shared = {
        "norm1_fm": fm(norm1_w[0]),
        "hw_fm": fm(headnorm_w[0]),
        "convw_fm": f(np.asarray(conv_w[0], dtype=np.float32).reshape(3, KC, 128).transpose(2, 1, 0).reshape(128, KC * 3)),
        "normf_b": f(np.broadcast_to(np.asarray(normf_w, dtype=np.float32)[None, :], (128, D))),
        "bif_b": f(np.broadcast_to(np.asarray(b_in[0], dtype=np.float32)[None, OI:OI + 16], (128, 16))),
        "ident_in": np.eye(128, dtype=np.float32),
        "tri_in": f(np.triu(np.ones((128, 128), dtype=np.float32))),
        "w_ada": f(w_ada[0]), "b_ada": f(np.asarray(b_ada[0])[None, :]),
        "w_in": f(w_in[0]), "b_in": f(np.asarray(b_in[0])[None, :]),
        "w_proj_a": f(w_proj_a[0]), "w_proj_b": f(w_proj_b[0]), "w_out": f(w_out[0]),
    }
    in_maps = []
    for core in range(8):
        b, hf = core // 2, core % 2
        m = dict(shared)
        m["x_main"] = f(x[b, hf * S_CORE:(hf + 1) * S_CORE])
        m["x_pre"] = f(x[b, 0:S_CORE])
        m["x_halo"] = f(x[b, S_CORE - 128:S_CORE])
        m["maskv"] = np.full((128, 1), float(hf), dtype=np.float32)
        m["c_fm"] = fm(np.asarray(c)[b])
        in_maps.append(m)
    return in_maps


_NC_CACHE = {}


def kernel(x, c, norm1_w, w_ada, b_ada, w_in, b_in, conv_w, headnorm_w,
           w_proj_a, w_proj_b, w_out, normf_w):
    in_maps = _prep_inputs(x, c, norm1_w, w_ada, b_ada, w_in, b_in, conv_w, headnorm_w,
                           w_proj_a, w_proj_b, w_out, normf_w)
    if "nc" not in _NC_CACHE:
        _NC_CACHE["nc"] = build_program()
    nc = _NC_CACHE["nc"]
    res = run_bass_kernel_spmd(nc, in_maps, core_ids=list(range(8)))
    outp = np.empty((4, 4096, D), dtype=np.float32)
    for core in range(8):
        b, hf = core // 2, core % 2
        outp[b, hf * S_CORE:(hf + 1) * S_CORE] = res.results[core]["out"]
    return outp
```

```python
import contextlib
import numpy as np
import concourse.bass as bass
import concourse.mybir as mybir
from concourse.bass_utils import run_bass_kernel_spmd

F32 = mybir.dt.float32
BF16 = mybir.dt.bfloat16
ALU = mybir.AluOpType
AF = mybir.ActivationFunctionType

D = 2048
KC = 16
S_CORE = 2048
TT = 1024
NT = TT // 128
NH = 8
EPS = 1e-6
OQ, OK_, OV, OO, OZA, OI, OF_, OU, OB, OC, OZB, OGA, OGB = (
    0, 1024, 2048, 4096, 6144, 8192, 8200, 8208, 10256, 12304, 14352, 16400, 18448)
INW = 20496
ENGS = ("sync", "scalar", "gpsimd", "vector", "tensor")


class Sem:
    def __init__(self, h):
        self.h = h
        self.n = 0


class Ev:
    __slots__ = ("sem", "val")

    def __init__(self, sem, val):
        self.sem = sem
        self.val = val


class Prog:
    def __init__(self, nc, stack):
        self.nc = nc
        self.stack = stack
        self.q = {e: [] for e in ENGS}
        self.waited = {e: {} for e in ENGS}
        self.esem = {}
        for e in ("scalar", "gpsimd", "vector", "tensor"):
            self.esem[e] = self.new_sem("es_" + e)

    def new_sem(self, name):
        return Sem(self.stack.enter_context(self.nc.semaphore(name)))

    def op(self, eng, fn, waits=(), sem=None, inc=1):
        ws = []
        for ev in _flat(waits):
            k = id(ev.sem)
            if self.waited[eng].get(k, 0) >= ev.val:
                continue
            self.waited[eng][k] = ev.val
            ws.append(ev)
        out = None
        if sem is not None:
            sem.n += inc
            out = Ev(sem, sem.n)
        self.q[eng].append((ws, fn, sem, inc))
        return out

    def dve(self, fn, waits=(), sig=True):
        return self.op("vector", fn, waits, self.esem["vector"] if sig else None)

    def act(self, fn, waits=(), sig=True):
        return self.op("scalar", fn, waits, self.esem["scalar"] if sig else None)

    def pe(self, fn, waits=(), sig=False):
        return self.op("tensor", fn, waits, self.esem["tensor"] if sig else None)

    def pool(self, fn, waits=(), sig=True):
        return self.op("gpsimd", fn, waits, self.esem["gpsimd"] if sig else None)

    def dma(self, queue, out, in_, waits, sem):
        return self.op(queue, REC.dma_start(out=out, in_=in_), waits, sem, 16)

    def emit(self, block):
        def runner(name):
            def f(e):
                for ws, fn, sem, inc in self.q[name]:
                    for w in ws:
                        e.wait_ge(w.sem.h, w.val)
                    if fn is None:
                        continue
                    ins = fn(e)
                    if sem is not None:
                        ins.then_inc(sem.h, inc)
            return f
        block.sync(runner("sync"))
        block.scalar(runner("scalar"))
        block.gpsimd(runner("gpsimd"))
        block.vector(runner("vector"))
        block.tensor(runner("tensor"))


class _Call:
    __slots__ = ("name", "args", "kwargs")

    def __init__(self, name, args, kwargs):
        self.name = name
        self.args = args
        self.kwargs = kwargs

    def __call__(self, e):
        return getattr(e, self.name)(*self.args, **self.kwargs)


class _Recorder:
    def __getattr__(self, name):
        return lambda *a, **kw: _Call(name, a, kw)


REC = _Recorder()


def _flat(x):
    if x is None:
        return
    if isinstance(x, Ev):
        yield x
        return
    for y in x:
        yield from _flat(y)


class Pool2:
    def __init__(self, bufs):
        self.bufs = bufs
        self.free = [[] for _ in bufs]
        self.i = 0

    def get(self):
        i = self.i
        self.i = (i + 1) % len(self.bufs)
        return i, self.bufs[i], self.free[i]

    def rel(self, i, *evs):
        self.free[i] = list(evs)


class _StopBuild(Exception):
    pass


def build_program(n_pre_st=2, n_main_st=2, stop=None):
    nc = bass.Bass("TRN2", target_bir_lowering=False)
    dram = lambda n, s, k="ExternalInput": nc.dram_tensor(n, s, F32, kind=k).ap()
    x_main = dram("x_main", [S_CORE, D])
    x_pre = dram("x_pre", [S_CORE, D])
    x_halo = dram("x_halo", [128, D])
    maskv = dram("maskv", [128, 1])
    c_fm = dram("c_fm", [128, KC])
    norm1_fm = dram("norm1_fm", [128, KC])
    hw_fm = dram("hw_fm", [128, KC])
    convw_fm = dram("convw_fm", [128, KC * 3])
    normf_b = dram("normf_b", [128, D])
    bif_b = dram("bif_b", [128, 16])
    ident_in = dram("ident_in", [128, 128])
    tri_in = dram("tri_in", [128, 128])
    w_ada = dram("w_ada", [D, 3 * D])
    b_ada = dram("b_ada", [1, 3 * D])
    w_in = dram("w_in", [D, INW])
    b_in = dram("b_in", [1, INW])
    w_pa = dram("w_proj_a", [D, D])
    w_pb = dram("w_proj_b", [D, D])
    w_o = dram("w_out", [D, D])
    out = dram("out", [S_CORE, D], "ExternalOutput")

    stack = contextlib.ExitStack()
    with stack:
        P = Prog(nc, stack)
        sb = lambda n, s, dt: stack.enter_context(nc.sbuf_tensor(n, s, dt))

        R1 = sb("R1", [128, 8192], F32)
        R2 = sb("R2", [128, 8192], F32)
        mbT = sb("mbT", [128, KC, TT], BF16)
        WS = [sb("ws%d" % i, [128, 17, 512], BF16) for i in range(3)]
        TR = sb("TR", [128, 6672], F32)
        Dst = sb("Dst", [128, NH, 257], F32)
        gate_b = sb("gate_b", [128, D], F32)
        normf_t = sb("normf_t", [128, D], F32)
        identb = sb("identb", [128, 128], BF16)
        maskf = sb("maskf", [128, 128], F32)
        onesf = sb("onesf", [128, 128], F32)
        onesrow = sb("onesrow", [1, 512], BF16)
        wif = sb("wif", [128, 17, 16], BF16)
        small = sb("small", [128, 512], F32)
        gates = sb("gates", [128, 6, NT * 8], F32)
        hist = sb("hist", [128, KC, 2], F32)

        hT = R1[:].bitcast(BF16).rearrange("p (a b) -> p a b", b=TT)
        hXT = R2[:].bitcast(BF16).rearrange("p (a b) -> p a b", b=TT)
        hThalo = mbT[:, 0:2, :].rearrange("p a b -> p (a b)").rearrange("p (a b) -> p a b", b=128)
        xo_lo = R1[:].rearrange("p (a b) -> p a b", b=D)
        xo_hi = R2[:].rearrange("p (a b) -> p a b", b=D)

        def TV(off_w, nwords, dt, inner=None):
            ap = TR[:, off_w:off_w + nwords]
            if dt == BF16:
                ap = ap.bitcast(BF16)
            if inner is not None:
                ap = ap.rearrange("p (a b) -> p a b", b=inner)
            return ap

        c_act = small[:, 0:16]
        n1w = small[:, 16:32]
        hw = small[:, 32:48]
        cw = small[:, 48:96].rearrange("p (a b) -> p a b", b=3)
        mod_fm = small[:, 96:128]
        A_fm = small[:, 128:144]
        maskt = small[:, 144:145]
        egc = small[:, 152:160]
        ssx = small[:, 160:176]
        rsx = small[:, 176:192]
        bifb = small[:, 192:208]
        uh = small[:, 208:212].rearrange("p (a b) -> p a b", b=2)
        rr = small[:, 224:256]
        g_if = sb("g_if", [128, NT, 16], F32)
        g_sp = gates[:, 1, :].rearrange("p (t c) -> p t c", c=8)
        g_a = gates[:, 2, :].rearrange("p (t c) -> p t c", c=8)
        g_ea = gates[:, 3, :].rearrange("p (t c) -> p t c", c=8)
        g_emb = gates[:, 4, :].rearrange("p (t c) -> p t c", c=8)
        g_eg = gates[:, 5, :].rearrange("p (t c) -> p t c", c=8)

        PS = [stack.enter_context(nc.psum_tensor("ps%d" % i, [128, 1024], F32)) for i in range(4)]
        pp = Pool2(PS)

        s_const = P.new_sem("s_const")
        s_ws = [P.new_sem("s_ws%d" % i) for i in range(3)]
        s_x = [P.new_sem("s_x%d" % i) for i in range(2)]
        s_xo = P.new_sem("s_xo")
        s_out = P.new_sem("s_out")
        wfree = [[] for _ in range(3)]
        wstate = {"i": 0}

        def wview(w):
            return w.rearrange("(kc p) n -> p kc n", p=128)

        def wload(w, c0, width, bias=None):
            i = wstate["i"]
            wstate["i"] = (i + 1) % 3
            slot = WS[i]
            for g4 in range(4):
                ev = P.dma("gpsimd", slot[:, 4 * g4:4 * g4 + 4, 0:width],
                           wview(w)[:, 4 * g4:4 * g4 + 4, c0:c0 + width], wfree[i] if g4 == 0 else (), s_ws[i])
            if bias is not None:
                ev = P.dma("gpsimd", slot[0:1, 16, 0:width], bias[0:1, c0:c0 + width], (), s_ws[i])
            return i, slot, ev

        def wrel(i, ev):
            wfree[i] = [ev]

        cev = []
        for dst, src in ((c_act, c_fm[:, :]), (n1w, norm1_fm[:, :]), (hw, hw_fm[:, :]),
                         (small[:, 48:96], convw_fm[:, :]), (maskt, maskv[:, :]),
                         (bifb, bif_b[:, :]), (maskf[:], tri_in[:, :]), (normf_t[:], normf_b[:, :])):
            cev.append(P.dma("sync", dst, src, (), s_const))
        c_loaded_h = cev[-1]
        s_constg = P.new_sem("s_constg")
        P.dma("gpsimd", identb[:], ident_in[:, :], (), s_constg)
        c_loaded_g = P.dma("gpsimd", wif[:, 0:KC, :], wview(w_in)[:, :, OI:OI + 16], (), s_constg)
        c_loaded = [c_loaded_h, c_loaded_g]
        e_m1 = P.dve(REC.memset(onesf[:], 1.0))
        e_m2 = P.dve(REC.memset(onesrow[:], 1.0))
        e_m3 = P.dve(REC.memset(Dst[:], 0.0))
        e_m4 = P.dve(REC.memset(egc, 1.0), c_loaded)
        e_m5 = P.dve(REC.memset(hist[:], 0.0))
        e_m6 = P.dve(REC.tensor_scalar(out=normf_t[:], in0=normf_t[:], scalar1=float(D ** 0.5),
                                               scalar2=None, op0=ALU.mult), c_loaded)
        e_m7 = P.dve(REC.tensor_scalar(out=hw, in0=hw, scalar1=16.0, scalar2=None, op0=ALU.mult),
                     c_loaded)
        cst_epsD = small[:, 256:257]
        cst_eps256 = small[:, 257:258]
        cst_mhalf = small[:, 258:259]
        e_m8 = P.dve(REC.memset(cst_epsD, float(D * EPS)))
        e_m9 = P.dve(REC.memset(cst_eps256, float(256 * EPS)))
        e_m10 = P.dve(REC.memset(cst_mhalf, -0.5))
        const_ev = c_loaded + [e_m1, e_m2, e_m3, e_m4, e_m5, e_m6, e_m7, e_m8, e_m9, e_m10]

        def pool_rsqrt(dst, src, cst, waits):
            e1 = P.pool(REC.tensor_tensor(out=dst, in0=src, in1=cst, op=ALU.add), list(waits) + const_ev)
            return P.pool(REC.tensor_tensor(out=dst, in0=dst, in1=cst_mhalf, op=ALU.pow), [e1])

        sgc = rr[:, 0:16]
        e = P.act(REC.activation(out=sgc, in_=c_act, func=AF.Sigmoid), c_loaded)
        e_cact = P.dve(REC.tensor_tensor(out=c_act, in0=c_act, in1=sgc, op=ALU.mult), [e] + c_loaded)
        cact_b = TV(0, 2048, F32, 128)
        evs = []
        for kc in range(KC):
            evs.append(P.dve(REC.tensor_copy(
                out=cact_b[:, kc, :], in_=c_act[:, kc:kc + 1].to_broadcast([128, 128])), [e_cact]))
        e_cb = evs[-1]
        s_ada = [P.new_sem("s_ada0"), P.new_sem("s_ada1")]
        ada_free = [[], []]
        mod_ps_i, mod_ps, mod_free = pp.get()
        last_mod_pe = None
        ada_slots = [R2[:, 0:4096].rearrange("p (a b) -> p a b", b=256),
                     R2[:, 4096:8192].rearrange("p (a b) -> p a b", b=256)]
        ada_bias = TV(2048, 512, F32, 256)
        e_mod = None
        for blk in range(24):
            if blk == 16:
                e_mod = P.dve(REC.tensor_copy(out=mod_fm, in_=mod_ps[:, 0:32]), [last_mod_pe])
                pp.rel(mod_ps_i, e_mod)
            si = blk % 2
            slot = ada_slots[si]
            c0 = blk * 256
            for g4 in range(4):
                ev = P.dma("sync", slot[:, 4 * g4:4 * g4 + 4, :], wview(w_ada)[:, 4 * g4:4 * g4 + 4, c0:c0 + 256],
                           ada_free[si] if g4 == 0 else (), s_ada[si])
            ev = P.dma("sync", ada_bias[0:1, si, :], b_ada[0:1, c0:c0 + 256], (), s_ada[si])
            if blk < 16:
                for j in range(2):
                    col = blk * 2 + j
                    for kc in range(KC):
                        P.pe(REC.matmul(
                            mod_ps[:, col:col + 1], lhsT=slot[:, kc, j * 128:(j + 1) * 128],
                            rhs=c_act[:, kc:kc + 1], start=(kc == 0), stop=False),
                            [ev, e_cact] + const_ev + mod_free if kc == 0 else ())
                    last = P.pe(REC.matmul(
                        mod_ps[:, col:col + 1], lhsT=ada_bias[0:1, si, j * 128:(j + 1) * 128],
                        rhs=onesf[0:1, 0:1], start=False, stop=True), (), sig=True)
                ada_free[si] = [last]
                last_mod_pe = last
            else:
                gi, gps, gfree = pp.get()
                gc0 = c0 - 4096
                for kc in range(KC):
                    P.pe(REC.matmul(
                        gps[:, 0:256], lhsT=cact_b[:, kc, :], rhs=slot[:, kc, :],
                        start=(kc == 0), stop=False), [ev, e_cb] + gfree if kc == 0 else ())
                last = P.pe(REC.matmul(
                    gps[:, 0:256], lhsT=onesf[0:1, :], rhs=ada_bias[0:1, si, :],
                    start=False, stop=True), (), sig=True)
                ada_free[si] = [last]
                ec = P.act(REC.activation(
                    out=gate_b[:, gc0:gc0 + 256], in_=gps[:, 0:256], func=AF.Identity), [last])
                pp.rel(gi, ec)
        e_ada_done = last
        e_A = P.dve(REC.scalar_tensor_tensor(
            out=A_fm, in0=mod_fm[:, 16:32], scalar=1.0, in1=n1w, op0=ALU.add, op1=ALU.mult), [e_mod] + c_loaded)
        e_A = P.dve(REC.tensor_scalar(out=A_fm, in0=A_fm, scalar1=float(D ** 0.5), scalar2=None,
                                              op0=ALU.mult), [e_A])
        shift_fm = mod_fm[:, 0:16]
        mod_ready = [e_A, e_mod]

        st = {"R1_free": [], "R2_free": [e_ada_done], "TR_free": [e_cb, e_ada_done],
              "mbT_free": [], "out_evs": []}

        def stage_A(xsrc, ntiles, dst, waits_dst, tr_waits):
            xt = [TV(0, 2048, F32), TV(2048, 2048, F32)]
            xn = [TV(4096, 1024, BF16), TV(5120, 1024, BF16)]
            xfree = [list(tr_waits), list(tr_waits)]
            nfree = [list(tr_waits), list(tr_waits)]
            done = []
            for t in range(ntiles):
                bi = t % 2
                e_ld = P.dma("sync", xt[bi], xsrc[t * 128:(t + 1) * 128, :], xfree[bi], s_x[bi])
                ssc = ssx[:, bi:bi + 1]
                rsc = rsx[:, bi:bi + 1]
                e_sq = P.act(REC.activation(
                    out=xn[bi], in_=xt[bi], func=AF.Square, accum_out=ssc), [e_ld] + nfree[bi])
                e_r2 = pool_rsqrt(rsc, ssc, cst_epsD, [e_sq] + xfree[bi])
                e_xn = P.dve(REC.tensor_scalar(
                    out=xn[bi], in0=xt[bi], scalar1=rsc, scalar2=None, op0=ALU.mult), [e_r2, e_sq, e_ld])
                xfree[bi] = [e_xn]
                pi, ps, pfree = pp.get()
                psb = ps[:].bitcast(BF16).rearrange("p (a b) -> p a b", b=128)
                for c in range(KC):
                    e_tr = P.pe(REC.transpose(
                        out=psb[:, c, :], in_=xn[bi][:, c * 128:(c + 1) * 128], identity=identb[:]),
                        [e_xn] + pfree + const_ev if c == 0 else (), sig=(c == KC - 1))
                nfree[bi] = [e_tr]
                evs = []
                for c in range(KC):
                    o = dst[:, c, t * 128:(t + 1) * 128]
                    if c < KC // 2:
                        evs.append(P.act(REC.activation(
                            out=o, in_=psb[:, c, :], func=AF.Identity,
                            scale=A_fm[:, c:c + 1], bias=shift_fm[:, c:c + 1]),
                            [e_tr] + mod_ready + waits_dst, sig=(c in (KC // 2 - 1, KC - 1))))
                    else:
                        evs.append(P.dve(REC.tensor_scalar(
                            out=o, in0=psb[:, c, :], scalar1=A_fm[:, c:c + 1], scalar2=shift_fm[:, c:c + 1],
                            op0=ALU.mult, op1=ALU.add), [e_tr] + mod_ready + waits_dst, sig=(c in (KC // 2 - 1, KC - 1))))
                pp.rel(pi, evs[KC - 1], evs[KC // 2 - 1])
                done += [evs[KC - 1], evs[KC // 2 - 1]]
            return done, xfree[0] + xfree[1] + nfree[0] + nfree[1]

        def stage_B(hT_ready, first):
            e_c = None
            if not first:
                e_c = P.dve(REC.tensor_copy(out=egc, in_=g_eg[:, NT - 1, :]), st["gates_last"])
            pi, ps, pfree = pp.get()
            psg = ps[:, 0:NT * 16].rearrange("p (t c) -> p t c", c=16)
            for t in range(NT):
                for kc in range(KC):
                    last = P.pe(REC.matmul(
                        psg[:, t, :], lhsT=hT[:, kc, t * 128:(t + 1) * 128], rhs=wif[:, kc, :],
                        start=(kc == 0), stop=(kc == KC - 1)),
                        hT_ready + pfree + const_ev if (t == 0 and kc == 0) else (),
                        sig=(t == NT - 1 and kc == KC - 1))
            wg = st.get("gates_last", []) + ([e_c] if e_c is not None else [])
            for t in range(NT):
                e1 = P.dve(REC.tensor_tensor(
                    out=g_if[:, t, :], in0=psg[:, t, :], in1=bifb, op=ALU.add), [last] + wg + const_ev)
            e2 = P.act(REC.activation(out=g_sp, in_=g_if[:, :, 8:16], func=AF.Exp, scale=-1.0), [e1] + wg)
            e3 = P.act(REC.activation(out=g_sp, in_=g_sp, func=AF.Ln, bias=1.0), [e2])
            psb_ = ps[:, 512:512 + NT * 8].rearrange("p (t c) -> p t c", c=8)
            psg_ = ps[:, 768:768 + NT * 8].rearrange("p (t c) -> p t c", c=8)
            for t in range(NT):
                P.pe(REC.matmul(psb_[:, t, :], lhsT=maskf[:], rhs=g_sp[:, t, :],
                                             start=True, stop=True), [e3, e1])
                e4 = P.pe(REC.matmul(psg_[:, t, :], lhsT=onesf[:], rhs=g_sp[:, t, :],
                                                  start=True, stop=True), (), sig=(t == NT - 1))
            e5 = P.dve(REC.tensor_tensor(out=g_a, in0=psb_, in1=g_if[:, :, 0:8], op=ALU.add), [e4, e1] + wg)
            e6 = P.act(REC.activation(out=g_ea, in_=g_a, func=AF.Exp), [e5] + wg)
            e7 = P.act(REC.activation(out=g_emb, in_=psb_, func=AF.Exp), [e4, e5] + wg)
            e8 = P.act(REC.activation(out=g_eg, in_=psg_, func=AF.Exp, scale=-1.0), [e4, e5] + wg)
            pp.rel(pi, e5, e8)
            return [e6, e7, e8]

        def fm_group(ps_ap, slot, c_in_slot, act_ap, ntok, first_waits, bias=True):
            last = None
            for h0 in range(0, ntok, 512):
                n = min(512, ntok - h0)
                for kc in range(KC):
                    last = P.pe(REC.matmul(
                        ps_ap[:, h0:h0 + n], lhsT=slot[:, kc, c_in_slot:c_in_slot + 128],
                        rhs=act_ap[:, kc, h0:h0 + n], start=(kc == 0), stop=(not bias and kc == KC - 1)),
                        first_waits if (kc == 0 and h0 == 0) else (),
                        sig=(not bias and kc == KC - 1 and h0 + n >= ntok))
                if bias:
                    last = P.pe(REC.matmul(
                        ps_ap[:, h0:h0 + n], lhsT=slot[0:1, 16, c_in_slot:c_in_slot + 128],
                        rhs=onesrow[0:1, 0:n], start=False, stop=True), (), sig=(h0 + n >= ntok))
            return last

        def tm_group(ps_ap, slot, width, act_tile, first_waits, bias=True):
            last = None
            for kc in range(KC):
                last = P.pe(REC.matmul(
                    ps_ap, lhsT=act_tile(kc), rhs=slot[:, kc, 0:width],
                    start=(kc == 0), stop=(not bias and kc == KC - 1)),
                    first_waits if kc == 0 else (), sig=(not bias and kc == KC - 1))
            if bias:
                last = P.pe(REC.matmul(
                    ps_ap, lhsT=onesrow[0:1, 0:128], rhs=slot[0:1, 16, 0:width],
                    start=False, stop=True), (), sig=True)
            return last

        def prefix_state(hT_ready, gate_ev):
            ktok = TV(0, 2048, BF16, 512)
            vp = TV(2048, 4 * NT * 129, BF16)
            vp = vp.rearrange("p (t h c) -> p t h c", h=4, c=258)
            tr_last = []
            for g in range(2):
                wi, slot, wev = wload(w_in, OK_ + g * 512, 512, b_in)
                evk = []
                for t2 in range(NT // 2):
                    pi, ps, pfree = pp.get()
                    for j in range(2):
                        t = t2 * 2 + j
                        last = tm_group(ps[:, j * 512:(j + 1) * 512], slot, 512,
                                        lambda kc, t=t: hT[:, kc, t * 128:(t + 1) * 128],
                                        [wev] + hT_ready + pfree + const_ev + st["TR_free"] + tr_last)
                    ek = P.act(REC.activation(
                        out=ktok[:, 2 * t2:2 * t2 + 2, :], in_=ps[:].rearrange("p (a b) -> p a b", b=512),
                        func=AF.Identity), [last] + st["TR_free"] + tr_last)
                    pp.rel(pi, ek)
                    evk.append(ek)
                wrel(wi, last)
                chk('pk')
                evv = []
                for j2 in range(2):
                    wi, slot, wev = wload(w_in, OV + g * 1024 + j2 * 512, 512, b_in)
                    for t in range(NT):
                        pi, ps, pfree = pp.get()
                        last = tm_group(ps[:, 0:512], slot, 512,
                                        lambda kc, t=t: hT[:, kc, t * 128:(t + 1) * 128],
                                        [wev] + hT_ready + pfree)
                        es = []
                        for hd in range(2):
                            hh = j2 * 2 + hd
                            head = g * 4 + hh
                            es.append(P.dve(REC.tensor_scalar(
                                out=vp[:, t, hh, 0:256], in0=ps[:, hd * 256:(hd + 1) * 256],
                                scalar1=g_ea[:, t, head:head + 1], scalar2=None, op0=ALU.mult),
                                [last] + gate_ev + st["TR_free"] + tr_last))
                        pp.rel(pi, *es)
                        evv += es
                    wrel(wi, last)
                for hh in range(4):
                    head = g * 4 + hh
                    evv.append(P.dve(REC.tensor_copy(
                        out=vp[:, :, hh, 256], in_=g_ea[:, :, head]), gate_ev + st["TR_free"] + tr_last))
                chk('pv')
                tr_last = []
                for t in range(NT):
                    for hh in range(4):
                        head = g * 4 + hh
                        pi, ps, pfree = pp.get()
                        em = P.pe(REC.matmul(
                            ps[:, 0:257], lhsT=ktok[:, t, hh * 128:(hh + 1) * 128], rhs=vp[:, t, hh, 0:257],
                            start=True, stop=True), evk + evv + pfree, sig=True)
                        sc = egc[:, head:head + 1] if t == 0 else g_eg[:, t - 1, head:head + 1]
                        ed = P.dve(REC.scalar_tensor_tensor(
                            out=Dst[:, head, :], in0=Dst[:, head, :], scalar=sc, in1=ps[:, 0:257],
                            op0=ALU.mult, op1=ALU.add), [em] + gate_ev + st["D_ev"][head])
                        st["D_ev"][head] = [ed]
                        pp.rel(pi, ed)
                        tr_last = [em]
            st["TR_free"] = tr_last
            st["hT_readers"] = tr_last

        def branch_B(hT_ready, first_main):
            cu = TV(0, 2 * 1026, F32, 1026)
            yy = TV(2052, 2 * 1024, F32, 1024)
            stmp = TV(4100, 2 * 1024, F32, 1024)
            trf = st["TR_free"]
            rd_ev = []
            last_pe = None
            for cbp in range(8):
                wi, slot, wev = wload(w_in, OU + cbp * 256, 256, b_in)
                e_u = []
                e_uh = []
                for j in range(2):
                    pi, ps, pfree = pp.get()
                    last = fm_group(ps, slot, j * 128, hT, TT, [wev] + hT_ready + pfree + const_ev)
                    if first_main:
                        pass
                    eu = P.act(REC.activation(out=cu[:, j, 2:1026], in_=ps[:, :], func=AF.Identity),
                               [last] + trf + rd_ev)
                    pp.rel(pi, eu)
                    e_u.append(eu)
                if first_main:
                    pi, ps, pfree = pp.get()
                    for j in range(2):
                        for kc in range(KC):
                            P.pe(REC.matmul(
                                ps[:, 2 * j:2 * j + 2], lhsT=slot[:, kc, j * 128:(j + 1) * 128],
                                rhs=hThalo[:, kc, 126:128], start=(kc == 0), stop=False),
                                st["halo_ready"] + pfree if (kc == 0 and j == 0) else ())
                        last = P.pe(REC.matmul(
                            ps[:, 2 * j:2 * j + 2], lhsT=slot[0:1, 16, j * 128:(j + 1) * 128],
                            rhs=onesrow[0:1, 0:2], start=False, stop=True), (), sig=True)
                    euh = P.dve(REC.tensor_copy(
                        out=uh, in_=ps[:, 0:4].rearrange("p (a b) -> p a b", b=2)), [last] + rd_ev)
                    pp.rel(pi, euh)
                    e_uh = [euh]
                wrel(wi, last)
                wi, slot, wev = wload(w_in, OC + cbp * 256, 256, b_in)
                e_cu = []
                for j in range(2):
                    cb = cbp * 2 + j
                    pi, ps, pfree = pp.get()
                    last = fm_group(ps, slot, j * 128, hT, TT, [wev] + pfree)
                    ec = P.dve(REC.tensor_tensor(
                        out=cu[:, j, 2:1026], in0=ps[:, :], in1=cu[:, j, 2:1026], op=ALU.mult), [last, e_u[j]])
                    pp.rel(pi, ec)
                    if not first_main:
                        eh = P.dve(REC.tensor_copy(out=cu[:, j, 0:2], in_=hist[:, cb, :]),
                                   rd_ev + trf + st["hist_ev"])
                        e_cu.append([ec, eh])
                    else:
                        e_cu.append([ec])
                if first_main:
                    pi, ps, pfree = pp.get()
                    for j in range(2):
                        for kc in range(KC):
                            P.pe(REC.matmul(
                                ps[:, 2 * j:2 * j + 2], lhsT=slot[:, kc, j * 128:(j + 1) * 128],
                                rhs=hThalo[:, kc, 126:128], start=(kc == 0), stop=False),
                                pfree if (kc == 0 and j == 0) else ())
                        last = P.pe(REC.matmul(
                            ps[:, 2 * j:2 * j + 2], lhsT=slot[0:1, 16, j * 128:(j + 1) * 128],
                            rhs=onesrow[0:1, 0:2], start=False, stop=True), (), sig=True)
                    eh1 = P.dve(REC.scalar_tensor_tensor(
                        out=cu[:, :, 0:2], in0=ps[:, 0:4].rearrange("p (a b) -> p a b", b=2),
                        scalar=maskt, in1=uh, op0=ALU.mult, op1=ALU.mult), [last] + e_uh + rd_ev + trf + const_ev)
                    pp.rel(pi, eh1)
                    for j in range(2):
                        e_cu[j].append(eh1)
                wrel(wi, last)
                e_y = []
                e_hist = []
                for j in range(2):
                    cb = cbp * 2 + j
                    y1 = P.dve(REC.tensor_scalar(
                        out=yy[:, j, :], in0=cu[:, j, 2:1026], scalar1=cw[:, cb, 2:3], scalar2=None,
                        op0=ALU.mult), e_cu[j] + rd_ev + const_ev)
                    y2 = P.dve(REC.scalar_tensor_tensor(
                        out=yy[:, j, :], in0=cu[:, j, 1:1025], scalar=cw[:, cb, 1:2], in1=yy[:, j, :],
                        op0=ALU.mult, op1=ALU.add), [y1])
                    y3 = P.dve(REC.scalar_tensor_tensor(
                        out=yy[:, j, :], in0=cu[:, j, 0:1024], scalar=cw[:, cb, 0:1], in1=yy[:, j, :],
                        op0=ALU.mult, op1=ALU.add), [y2])
                    eh = P.dve(REC.tensor_copy(out=hist[:, cb, :], in_=cu[:, j, 1024:1026]),
                               e_cu[j] + st["hist_ev"])
                    e_y.append(y3)
                    e_hist.append(eh)
                wi, slot, wev = wload(w_in, OB + cbp * 256, 256, b_in)
                e_b = []
                for j in range(2):
                    pi, ps, pfree = pp.get()
                    last = fm_group(ps, slot, j * 128, hT, TT, [wev] + pfree)
                    eb = P.dve(REC.tensor_tensor(
                        out=yy[:, j, :], in0=ps[:, :], in1=yy[:, j, :], op=ALU.mult), [last, e_y[j]])
                    pp.rel(pi, eb)
                    e_b.append(eb)
                wrel(wi, last)
                wi, slot, wev = wload(w_in, OZB + cbp * 256, 256, b_in)
                new_rd = []
                for j in range(2):
                    cb = cbp * 2 + j
                    pi, ps, pfree = pp.get()
                    last = fm_group(ps, slot, j * 128, hT, TT, [wev] + pfree)
                    es = P.act(REC.activation(out=stmp[:, j, :], in_=ps[:, :], func=AF.Sigmoid),
                               [last] + rd_ev + trf)
                    e1 = P.dve(REC.tensor_tensor(
                        out=yy[:, j, :], in0=yy[:, j, :], in1=stmp[:, j, :], op=ALU.mult), [es, e_b[j]])
                    e2 = P.dve(REC.tensor_tensor(
                        out=hXT[:, cb, :], in0=ps[:, :], in1=yy[:, j, :], op=ALU.mult),
                        [e1, last] + st["R2_free"])
                    pp.rel(pi, e2, es)
                    new_rd += [e2]
                wrel(wi, last)
                last_pe = last
                rd_ev = new_rd + e_hist
                st["hist_ev"] = e_hist
            st["TR_free"] = rd_ev
            return rd_ev

        def merge_stage(w_proj, og, actT, act_ready, hT_ready, accumulate):
            stmp = TV(0, 4096, F32, 1024)
            trf = st["TR_free"]
            rd = [[], [], [], []]
            outs = []
            last = lastG = None
            for db in range(4):
                wiG, slotG, wevG = wload(w_in, og + db * 512, 512, b_in)
                wiP, slotP, wevP = wload(w_proj, db * 512, 512, None)
                es_l = []
                for j in range(4):
                    pi, psG, pfree = pp.get()
                    lastG = fm_group(psG, slotG, j * 128, hT, TT, [wevG] + hT_ready + pfree + const_ev)
                    es = P.act(REC.activation(out=stmp[:, j, :], in_=psG[:, :], func=AF.Sigmoid),
                               [lastG] + rd[j] + trf)
                    pp.rel(pi, es)
                    es_l.append(es)
                wrel(wiG, lastG)
                for j in range(4):
                    blk = db * 4 + j
                    pi, psY, pfree = pp.get()
                    last = fm_group(psY, slotP, j * 128, actT, TT, [wevP] + act_ready + pfree, bias=False)
                    if not accumulate:
                        eo = P.dve(REC.tensor_tensor(
                            out=mbT[:, blk, :], in0=psY[:, :], in1=stmp[:, j, :], op=ALU.mult),
                            [last, es_l[j]] + st["mbT_free"])
                        pp.rel(pi, eo)
                    else:
                        e1 = P.dve(REC.tensor_tensor(
                            out=stmp[:, j, :], in0=psY[:, :], in1=stmp[:, j, :], op=ALU.mult), [last, es_l[j]])
                        pp.rel(pi, e1)
                        eo = P.dve(REC.tensor_tensor(
                            out=mbT[:, blk, :], in0=stmp[:, j, :], in1=mbT[:, blk, :], op=ALU.add), [e1])
                    rd[j] = [eo]
                    outs.append(eo)
                wrel(wiP, last)
            st["TR_free"] = rd[0] + rd[1] + rd[2] + rd[3]
            return outs, [last, lastG]

        def heads_stage(hT_ready, gate_ev):
            qT = TV(0, 512, BF16)
            kT = TV(512, 512, BF16)
            ktok = TV(1024, 512, BF16, 128)
            vp = TV(1536, NT * 129, BF16, 258)
            sgo = TV(2568, 1024, BF16, 256)
            zsl = TV(3592, 1024, BF16, 1024)
            zsg = TV(4616, 1024, F32)
            PT = [TV(5640, 64, BF16), TV(5704, 64, BF16)]
            Sbf = [TV(5768, 130, BF16), TV(5898, 130, BF16)]
            hs = [TV(6028, 257, F32), TV(6285, 257, F32)]
            hn = [TV(6542, 128, BF16), TV(4616 + 200, 128, BF16)]
            sqjunk = TV(4616, 129, BF16)[:, 0:257]
            trf = st["TR_free"]
            e_hc = [P.dve(REC.memset(hs[i][:, 256:257], float((256 * EPS) ** 0.5)), trf) for i in range(2)]
            prev = list(trf) + e_hc
            R2w = st["R2_free"]
            fin = []
            for h in range(NH):
                if h == 0:
                    qk_pref = [wload(w_in, OQ, 128, b_in), wload(w_in, OK_, 128, b_in)]
                wi, slot, wev = qk_pref[0]
                pi, ps, pfree = pp.get()
                last = fm_group(ps, slot, 0, hT, TT, [wev] + hT_ready + pfree + const_ev)
                e_q = P.act(REC.activation(out=qT, in_=ps[:, :], func=AF.Identity,
                                                          scale=float(128 ** -0.5)), [last] + prev)
                pp.rel(pi, e_q)
                wrel(wi, last)
                wi, slot, wev = qk_pref[1]
                pi, ps, pfree = pp.get()
                last = fm_group(ps, slot, 0, hT, TT, [wev] + pfree)
                e_k = P.dve(REC.tensor_copy(out=kT, in_=ps[:, :]), [last] + prev)
                pp.rel(pi, e_k)
                wrel(wi, last)
                pi, ps, pfree = pp.get()
                psb = ps[:, 0:512].bitcast(BF16).rearrange("p (a b) -> p a b", b=128)
                for t in range(NT):
                    e_tr = P.pe(REC.transpose(
                        out=psb[:, t, :], in_=kT[:, t * 128:(t + 1) * 128], identity=identb[:]),
                        [e_k] + pfree if t == 0 else (), sig=(t == NT - 1))
                e_kt = P.act(REC.activation(out=ktok, in_=psb, func=AF.Identity), [e_tr] + prev)
                pp.rel(pi, e_kt)
                wi, slot, wev = wload(w_in, OV + h * 256, 256, b_in)
                e_v = []
                for t4 in range(NT // 4):
                    pi, ps, pfree = pp.get()
                    for j in range(4):
                        t = t4 * 4 + j
                        last = tm_group(ps[:, j * 256:(j + 1) * 256], slot, 256,
                                        lambda kc, t=t: hT[:, kc, t * 128:(t + 1) * 128], [wev] + pfree)
                    es = []
                    for j in range(4):
                        t = t4 * 4 + j
                        fn = REC.tensor_scalar(
                            out=vp[:, t, 0:256], in0=ps[:, j * 256:(j + 1) * 256],
                            scalar1=g_ea[:, t, h:h + 1], scalar2=None, op0=ALU.mult)
                        es.append(P.dve(fn, [last] + gate_ev + prev))
                    pp.rel(pi, *es)
                    e_v += es
                wrel(wi, last)
                e_v.append(P.dve(REC.tensor_copy(out=vp[:, :, 256], in_=g_ea[:, :, h]), gate_ev + prev))
                wi, slot, wev = wload(w_in, OO + h * 256, 256, b_in)
                e_o = []
                for t4 in range(NT // 4):
                    pi, ps, pfree = pp.get()
                    for j in range(4):
                        t = t4 * 4 + j
                        last = tm_group(ps[:, j * 256:(j + 1) * 256], slot, 256,
                                        lambda kc, t=t: hT[:, kc, t * 128:(t + 1) * 128], [wev] + pfree)
                    eo = P.act(REC.activation(
                        out=sgo[:, t4 * 4:(t4 + 1) * 4, :], in_=ps[:].rearrange("p (a b) -> p a b", b=256),
                        func=AF.Sigmoid), [last] + prev)
                    pp.rel(pi, eo)
                    e_o.append(eo)
                wrel(wi, last)
                wi, slot, wev = wload(w_in, OZA + h * 256, 256, b_in)
                e_z = []
                ezprev = list(prev)
                for j in range(2):
                    pi, ps, pfree = pp.get()
                    last = fm_group(ps, slot, j * 128, hT, TT, [wev] + pfree)
                    es = P.act(REC.activation(out=zsg, in_=ps[:, :], func=AF.Sigmoid), [last] + ezprev)
                    ez = P.dve(REC.tensor_tensor(
                        out=zsl[:, j, :], in0=ps[:, :], in1=zsg, op=ALU.mult), [es, last] + prev)
                    pp.rel(pi, ez)
                    ezprev = [ez]
                    e_z.append(ez)
                wrel(wi, last)
                if h + 1 < NH:
                    qk_pref = [wload(w_in, OQ + (h + 1) * 128, 128, b_in),
                               wload(w_in, OK_ + (h + 1) * 128, 128, b_in)]
                e_s = P.act(REC.activation(out=Sbf[0][:, 0:257], in_=Dst[:, h, :], func=AF.Identity,
                                                   scale=egc[:, h:h + 1]), st["D_ev"][h] + gate_ev + prev + const_ev)
                sbf_ev = [e_s]
                pt_free = [list(prev), list(prev)]
                sbf_free = [list(prev), list(prev)]
                hs_free = [list(prev), list(prev)]
                hn_free = [list(prev), list(prev)]
                pend = None
                head_done = []

                def finish(pd):
                    (t, k, piB, psB, e_num, e_hsq, hn_w) = pd
                    psT = psB[:, 260:388].bitcast(BF16).rearrange("p (a b) -> p a b", b=128)
                    for j in range(2):
                        e_tr = P.pe(REC.transpose(
                            out=psT[:, j, :], in_=hn[k][:, j * 128:(j + 1) * 128], identity=identb[:]),
                            [hn_w] if j == 0 else (), sig=(j == 1))
                    evs = []
                    for j in range(2):
                        ch = 2 * h + j
                        evs.append(P.dve(REC.scalar_tensor_tensor(
                            out=hXT[:, ch, t * 128:(t + 1) * 128], in0=psT[:, j, :], scalar=hw[:, ch:ch + 1],
                            in1=zsl[:, j, t * 128:(t + 1) * 128], op0=ALU.mult, op1=ALU.mult),
                            [e_tr] + e_z + R2w + const_ev))
                    pp.rel(piB, *evs)
                    return [e_tr], evs

                for t in range(NT):
                    k = t % 2
                    piA, psA, pfreeA = pp.get()
                    qs = qT[:, t * 128:(t + 1) * 128]
                    ks = kT[:, t * 128:(t + 1) * 128]
                    e_sc = P.pe(REC.matmul(
                        psA[:, 0:128], lhsT=ks, rhs=qs, start=True, stop=True),
                        [e_q, e_k] + pfreeA, sig=True)
                    e_dc = P.pe(REC.matmul(
                        psA[:, 512:769], lhsT=ktok[:, t, :], rhs=vp[:, t, 0:257], start=True, stop=True),
                        [e_kt] + e_v, sig=True)
                    e_pt = P.dve(REC.tensor_tensor(
                        out=PT[k], in0=psA[:, 0:128], in1=maskf[:], op=ALU.mult), [e_sc] + pt_free[k] + const_ev)
                    sc = egc[:, h:h + 1] if t == 0 else g_eg[:, t - 1, h:h + 1]
                    e_d = P.dve(REC.scalar_tensor_tensor(
                        out=Dst[:, h, :], in0=Dst[:, h, :], scalar=sc, in1=psA[:, 512:769],
                        op0=ALU.mult, op1=ALU.add), [e_dc] + st["D_ev"][h] + sbf_ev + gate_ev)
                    st["D_ev"][h] = [e_d]
                    pp.rel(piA, e_pt, e_d)
                    piB, psB, pfreeB = pp.get()
                    P.pe(REC.matmul(
                        psB[:, 0:257], lhsT=PT[k], rhs=vp[:, t, 0:257], start=True, stop=False),
                        [e_pt] + pfreeB)
                    e_num = P.pe(REC.matmul(
                        psB[:, 0:257], lhsT=qs, rhs=Sbf[k][:, 0:257], start=False, stop=True),
                        sbf_ev, sig=True)
                    pt_free[k] = [e_num]
                    sbf_free[k] = [e_num]
                    if t + 1 < NT:
                        e_s = P.act(REC.activation(
                            out=Sbf[1 - k][:, 0:257], in_=Dst[:, h, :], func=AF.Identity,
                            scale=g_eg[:, t, h:h + 1]), [e_d] + sbf_free[1 - k] + gate_ev)
                        sbf_ev = [e_s]
                    if pend is not None:
                        trs, evs = finish(pend)
                        hn_free[pend[1]] = trs
                        head_done = evs
                    rc = rr[:, (h % 2) * 16 + 2 * t:(h % 2) * 16 + 2 * t + 1]
                    sc2 = rr[:, (h % 2) * 16 + 2 * t + 1:(h % 2) * 16 + 2 * t + 2]
                    e_r0 = P.act(REC.activation(
                        out=rc, in_=psB[:, 256:257], func=AF.Abs), [e_num])
                    e_r = P.dve(REC.tensor_scalar(
                        out=rc, in0=rc, scalar1=g_emb[:, t, h:h + 1], scalar2=None, op0=ALU.max),
                        [e_r0] + gate_ev)
                    e_r = P.dve(REC.reciprocal(out=rc, in_=rc), [e_r])
                    e_hs = P.dve(REC.scalar_tensor_tensor(
                        out=hs[k][:, 0:256], in0=psB[:, 0:256], scalar=rc, in1=sgo[:, t, :],
                        op0=ALU.mult, op1=ALU.mult), [e_r] + e_o + hs_free[k])
                    e_sq = P.act(REC.activation(
                        out=sqjunk, in_=hs[k], func=AF.Square, accum_out=sc2), [e_hs] + e_z)
                    e_r2 = P.pool(REC.tensor_tensor(out=sc2, in0=sc2, in1=cst_mhalf, op=ALU.pow),
                                  [e_sq] + const_ev)
                    e_hn = P.act(REC.activation(
                        out=hn[k], in_=hs[k][:, 0:256], func=AF.Identity, scale=sc2),
                        [e_r2, e_sq] + hn_free[k] + e_z)
                    hs_free[k] = [e_hn]
                    pend = (t, k, piB, psB, e_num, e_sq, e_hn)
                trs, evs = finish(pend)
                head_done = evs
                prev = evs + trs + [e_hn, e_num, e_d]
                fin = evs
            st["TR_free"] = prev
            return fin, prev

        def final_stage(tok0, mb_ready, dead_ev):
            tmp = [TV(0, 512, F32), TV(512, 512, F32)]
            trf = st["TR_free"]
            xe = []
            for t in range(NT):
                dst = (xo_lo if t < 4 else xo_hi)[:, t % 4, :]
                xe.append(P.dma("sync", dst, x_main[tok0 + t * 128:tok0 + (t + 1) * 128, :], dead_ev, s_xo))
            x_all = xe[-1]
            tfree = [list(trf), list(trf)]
            acc_ev = [[] for _ in range(NT)]
            last = None
            for db in range(4):
                wi, slot, wev = wload(w_o, db * 512, 512, None)
                for t in range(NT):
                    xo_t = (xo_lo if t < 4 else xo_hi)[:, t % 4, db * 512:(db + 1) * 512]
                    k = t % 2
                    pi, ps, pfree = pp.get()
                    last = tm_group(ps[:, 0:512], slot, 512,
                                    lambda kc, t=t: mbT[:, kc, t * 128:(t + 1) * 128],
                                    [wev] + mb_ready + pfree + const_ev, bias=False)
                    e1 = P.dve(REC.tensor_tensor(
                        out=tmp[k], in0=ps[:, 0:512], in1=gate_b[:, db * 512:(db + 1) * 512], op=ALU.mult),
                        [last] + tfree[k])
                    pp.rel(pi, e1)
                    e2 = P.dve(REC.tensor_tensor(
                        out=xo_t, in0=tmp[k], in1=xo_t, op=ALU.add), [e1, x_all])
                    tfree[k] = [e2]
                    acc_ev[t] = [e2]
                wrel(wi, last)
            st["mbT_free"] = [last]
            chk('finA')
            outs = []
            for t in range(NT):
                xo_t = (xo_lo if t < 4 else xo_hi)[:, t % 4, :]
                ssc = ssx[:, 2 + (t % 2):3 + (t % 2)]
                junk = TV(1024, 1024, BF16)
                e_sq = P.act(REC.activation(
                    out=junk, in_=xo_t, func=AF.Square, accum_out=ssc), acc_ev[t] + outs[-2:])
                e_r2 = pool_rsqrt(ssc, ssc, cst_epsD, [e_sq])
                e_o = P.dve(REC.scalar_tensor_tensor(
                    out=xo_t, in0=xo_t, scalar=ssc, in1=normf_t[:], op0=ALU.mult, op1=ALU.mult),
                    [e_r2, e_sq] + const_ev)
                e_st = P.dma("sync", out[tok0 + t * 128:tok0 + (t + 1) * 128, :], xo_t, [e_o], s_out)
                outs.append(e_o)
                st["out_last"] = e_st
            st["TR_free"] = tfree[0] + tfree[1]
            st["R1_free"] = [st["out_last"]]
            st["R2_free"] = [st["out_last"]]

        def chk(tag):
            if stop == tag:
                raise _StopBuild()

        def main_emit():
            st["D_ev"] = [[e_m3] for _ in range(NH)]
            st["hist_ev"] = [e_m5]
            st["gates_last"] = []
            first = True
            for s_i in range(n_pre_st):
                hev, trl = stage_A(x_pre[s_i * TT:(s_i + 1) * TT, :], NT, hT,
                                   st["R1_free"] + st.get("hT_readers", []), st["TR_free"])
                st["TR_free"] = trl
                gev = stage_B(hev, first)
                chk('preB')
                first = False
                prefix_state(hev, gev)
                chk('prefix1')
                st["gates_last"] = st["TR_free"] + [x for h in range(NH) for x in st["D_ev"][h]]
            if n_pre_st > 0:
                for h in range(NH):
                    ed = P.dve(REC.tensor_scalar(
                        out=Dst[:, h, :], in0=Dst[:, h, :], scalar1=g_eg[:, NT - 1, h:h + 1], scalar2=maskt,
                        op0=ALU.mult, op1=ALU.mult), st["D_ev"][h] + const_ev)
                    st["D_ev"][h] = [ed]
                st["gates_last"] = st["gates_last"] + [ed]
            hev, trl = stage_A(x_halo, 1, hThalo, [], st["TR_free"])
            st["TR_free"] = trl
            st["halo_ready"] = hev
            chk('halo')
            for s_i in range(n_main_st):
                tok0 = s_i * TT
                hev, trl = stage_A(x_main[tok0:tok0 + TT, :], NT, hT,
                                   st["R1_free"] + st.get("hT_readers", []), st["TR_free"])
                st["TR_free"] = trl
                chk('A')
                gev = stage_B(hev, first and s_i == 0)
                chk('B')
                if s_i == 0 and n_pre_st > 0:
                    e_fix = P.dve(REC.memset(egc, 1.0), gev + st["gates_last"])
                    gev = gev + [e_fix]
                hb_ev = branch_B(hev, s_i == 0)
                chk('brB')
                mb_ev, pe_last = merge_stage(w_pb, OGB, hXT, hb_ev, hev, False)
                st["R2_free"] = st["R2_free"] + [pe_last[0]]
                chk('mergeB')
                ha_ev, tr_ev = heads_stage(hev, gev)
                chk('heads')
                st["gates_last"] = tr_ev + [x for h in range(NH) for x in st["D_ev"][h]]
                mb_ev2, pe_last2 = merge_stage(w_pa, OGA, hXT, ha_ev, hev, True)
                chk('mergeA')
                final_stage(tok0, mb_ev2, pe_last2 + mb_ev2)
                st["hT_readers"] = []
        try:
            chk('ada')
            main_emit()
            P.op("sync", None, [st["out_last"]])
        except _StopBuild:
            lasts = [Ev(P.esem[e], P.esem[e].n) for e in ("scalar", "gpsimd", "vector", "tensor") if P.esem[e].n > 0]
            lasts += [Ev(sm, sm.n) for sm in s_ws if sm.n > 0]
            ed = P.dma("sync", out[0:128, :], gate_b[:], lasts, s_out)
            mbw = mbT[:].rearrange("p a b -> p (a b)").bitcast(F32)
            for r4 in range(4):
                ed = P.dma("sync", out[128 * (r4 + 1):128 * (r4 + 2), :], mbw[:, r4 * 2048:(r4 + 1) * 2048], lasts, s_out)
            ed = P.dma("sync", out[640:768, :], Dst[:].rearrange("p a b -> p (a b)")[:, 0:2048], lasts, s_out)
            ed = P.dma("sync", out[768:896, 0:384], gates[:].rearrange("p a b -> p (a b)"), lasts, s_out)
            for r4 in range(4):
                ed = P.dma("sync", out[896 + 128 * r4:1024 + 128 * r4, :], R1[:, r4 * 2048:(r4 + 1) * 2048], lasts, s_out)
            P.op("sync", None, [ed])

        with nc.Block() as block:
            P.emit(block)
    return nc


def _prep_inputs(x, c, norm1_w, w_ada, b_ada, w_in, b_in, conv_w, headnorm_w,
                 w_proj_a, w_proj_b, w_out, normf_w):
    f = lambda a: np.ascontiguousarray(np.asarray(a, dtype=np.float32))
    x = f(x)
    fm = lambda v: f(np.asarray(v, dtype=np.float32).reshape(KC, 128).T)
    shared = {
        "norm1_fm": fm(norm1_w[0]),
        "hw_fm": fm(headnorm_w[0]),
        "convw_fm": f(np.asarray(conv_w[0], dtype=np.float32).reshape(3, KC, 128).transpose(2, 1, 0).reshape(128, KC * 3)),
        "normf_b": f(np.broadcast_to(np.asarray(normf_w, dtype=np.float32)[None, :], (128, D))),
        "bif_b": f(np.broadcast_to(np.asarray(b_in[0], dtype=np.float32)[None, OI:OI + 16], (128, 16))),
        "ident_in": np.eye(128, dtype=np.float32),
        "tri_in": f(np.triu(np.ones((128, 128), dtype=np.float32))),
        "w_ada": f(w_ada[0]), "b_ada": f(np.asarray(b_ada[0])[None, :]),
        "w_in": f(w_in[0]), "b_in": f(np.asarray(b_in[0])[None, :]),
        "w_proj_a": f(w_proj_a[0]), "w_proj_b": f(w_proj_b[0]), "w_out": f(w_out[0]),
    }
    in_maps = []
    for core in range(8):
        b, hf = core // 2, core % 2
        m = dict(shared)
        m["x_main"] = f(x[b, hf * S_CORE:(hf + 1) * S_CORE])
        m["x_pre"] = f(x[b, 0:S_CORE])
        m["x_halo"] = f(x[b, S_CORE - 128:S_CORE])
        m["maskv"] = np.full((128, 1), float(hf), dtype=np.float32)
        m["c_fm"] = fm(np.asarray(c)[b])
        in_maps.append(m)
    return in_maps


_NC_CACHE = {}


def kernel(x, c, norm1_w, w_ada, b_ada, w_in, b_in, conv_w, headnorm_w,
           w_proj_a, w_proj_b, w_out, normf_w):
    in_maps = _prep_inputs(x, c, norm1_w, w_ada, b_ada, w_in, b_in, conv_w, headnorm_w,
                           w_proj_a, w_proj_b, w_out, normf_w)
    if "nc" not in _NC_CACHE:
        _NC_CACHE["nc"] = build_program()
    nc = _NC_CACHE["nc"]
    res = run_bass_kernel_spmd(nc, in_maps, core_ids=list(range(8)))
    outp = np.empty((4, 4096, D), dtype=np.float32)
    for core in range(8):
        b, hf = core // 2, core % 2
        outp[b, hf * S_CORE:(hf + 1) * S_CORE] = res.results[core]["out"]
    return outp
```

```python
import contextlib
import numpy as np
import concourse.bass as bass
import concourse.mybir as mybir
from concourse.bass_utils import run_bass_kernel_spmd

F32 = mybir.dt.float32
BF16 = mybir.dt.bfloat16
ALU = mybir.AluOpType
AF = mybir.ActivationFunctionType

D = 2048
KC = 16
S_CORE = 2048
TT = 1024
NT = TT // 128
NH = 8
EPS = 1e-6
OQ, OK_, OV, OO, OZA, OI, OF_, OU, OB, OC, OZB, OGA, OGB = (
    0, 1024, 2048, 4096, 6144, 8192, 8200, 8208, 10256, 12304, 14352, 16400, 18448)
INW = 20496
ENGS = ("sync", "scalar", "gpsimd", "vector", "tensor")


class Sem:
    def __init__(self, h):
        self.h = h
        self.n = 0


class Ev:
    __slots__ = ("sem", "val")

    def __init__(self, sem, val):
        self.sem = sem
        self.val = val


class Prog:
    def __init__(self, nc, stack):
        self.nc = nc
        self.stack = stack
        self.q = {e: [] for e in ENGS}
        self.waited = {e: {} for e in ENGS}
        self.esem = {}
        for e in ("scalar", "gpsimd", "vector", "tensor"):
            self.esem[e] = self.new_sem("es_" + e)

    def new_sem(self, name):
        return Sem(self.stack.enter_context(self.nc.semaphore(name)))

    def op(self, eng, fn, waits=(), sem=None, inc=1):
        ws = []
        for ev in _flat(waits):
            k = id(ev.sem)
            if self.waited[eng].get(k, 0) >= ev.val:
                continue
            self.waited[eng][k] = ev.val
            ws.append(ev)
        out = None
        if sem is not None:
            sem.n += inc
            out = Ev(sem, sem.n)
        self.q[eng].append((ws, fn, sem, inc))
        return out

    def dve(self, fn, waits=(), sig=True):
        return self.op("vector", fn, waits, self.esem["vector"] if sig else None)

    def act(self, fn, waits=(), sig=True):
        return self.op("scalar", fn, waits, self.esem["scalar"] if sig else None)

    def pe(self, fn, waits=(), sig=False):
        return self.op("tensor", fn, waits, self.esem["tensor"] if sig else None)

    def pool(self, fn, waits=(), sig=True):
        return self.op("gpsimd", fn, waits, self.esem["gpsimd"] if sig else None)

    def dma(self, queue, out, in_, waits, sem):
        return self.op(queue, REC.dma_start(out=out, in_=in_), waits, sem, 16)

    def emit(self, block):
        def runner(name):
            def f(e):
                for ws, fn, sem, inc in self.q[name]:
                    for w in ws:
                        e.wait_ge(w.sem.h, w.val)
                    if fn is None:
                        continue
                    ins = fn(e)
                    if sem is not None:
                        ins.then_inc(sem.h, inc)
            return f
        block.sync(runner("sync"))
        block.scalar(runner("scalar"))
        block.gpsimd(runner("gpsimd"))
        block.vector(runner("vector"))
        block.tensor(runner("tensor"))


class _Call:
    __slots__ = ("name", "args", "kwargs")

    def __init__(self, name, args, kwargs):
        self.name = name
        self.args = args
        self.kwargs = kwargs

    def __call__(self, e):
        return getattr(e, self.name)(*self.args, **self.kwargs)


class _Recorder:
    def __getattr__(self, name):
        return lambda *a, **kw: _Call(name, a, kw)


REC = _Recorder()


def _flat(x):
    if x is None:
        return
    if isinstance(x, Ev):
        yield x
        return
    for y in x:
        yield from _flat(y)


class Pool2:
    def __init__(self, bufs):
        self.bufs = bufs
        self.free = [[] for _ in bufs]
        self.i = 0

    def get(self):
        i = self.i
        self.i = (i + 1) % len(self.bufs)
        return i, self.bufs[i], self.free[i]

    def rel(self, i, *evs):
        self.free[i] = list(evs)


class _StopBuild(Exception):
    pass


def build_program(n_pre_st=2, n_main_st=2, stop=None):
    nc = bass.Bass("TRN2", target_bir_lowering=False)
    dram = lambda n, s, k="ExternalInput": nc.dram_tensor(n, s, F32, kind=k).ap()
    x_main = dram("x_main", [S_CORE, D])
    x_pre = dram("x_pre", [S_CORE, D])
    x_halo = dram("x_halo", [128, D])
    maskv = dram("maskv", [128, 1])
    c_fm = dram("c_fm", [128, KC])
    norm1_fm = dram("norm1_fm", [128, KC])
    hw_fm = dram("hw_fm", [128, KC])
    convw_fm = dram("convw_fm", [128, KC * 3])
    normf_b = dram("normf_b", [128, D])
    bif_b = dram("bif_b", [128, 16])
    ident_in = dram("ident_in", [128, 128])
    tri_in = dram("tri_in", [128, 128])
    w_ada = dram("w_ada", [D, 3 * D])
    b_ada = dram("b_ada", [1, 3 * D])
    w_in = dram("w_in", [D, INW])
    b_in = dram("b_in", [1, INW])
    w_pa = dram("w_proj_a", [D, D])
    w_pb = dram("w_proj_b", [D, D])
    w_o = dram("w_out", [D, D])
    out = dram("out", [S_CORE, D], "ExternalOutput")

    stack = contextlib.ExitStack()
    with stack:
        P = Prog(nc, stack)
        sb = lambda n, s, dt: stack.enter_context(nc.sbuf_tensor(n, s, dt))

        R1 = sb("R1", [128, 8192], F32)
        R2 = sb("R2", [128, 8192], F32)
        mbT = sb("mbT", [128, KC, TT], BF16)
        WS = [sb("ws%d" % i, [128, 17, 512], BF16) for i in range(3)]
        TR = sb("TR", [128, 6672], F32)
        Dst = sb("Dst", [128, NH, 257], F32)
        gate_b = sb("gate_b", [128, D], F32)
        normf_t = sb("normf_t", [128, D], F32)
        identb = sb("identb", [128, 128], BF16)
        maskf = sb("maskf", [128, 128], F32)
        onesf = sb("onesf", [128, 128], F32)
        onesrow = sb("onesrow", [1, 512], BF16)
        wif = sb("wif", [128, 17, 16], BF16)
        small = sb("small", [128, 512], F32)
        gates = sb("gates", [128, 6, NT * 8], F32)
        hist = sb("hist", [128, KC, 2], F32)

        hT = R1[:].bitcast(BF16).rearrange("p (a b) -> p a b", b=TT)
        hXT = R2[:].bitcast(BF16).rearrange("p (a b) -> p a b", b=TT)
        hThalo = mbT[:, 0:2, :].rearrange("p a b -> p (a b)").rearrange("p (a b) -> p a b", b=128)
        xo_lo = R1[:].rearrange("p (a b) -> p a b", b=D)
        xo_hi = R2[:].rearrange("p (a b) -> p a b", b=D)

        def TV(off_w, nwords, dt, inner=None):
            ap = TR[:, off_w:off_w + nwords]
            if dt == BF16:
                ap = ap.bitcast(BF16)
            if inner is not None:
                ap = ap.rearrange("p (a b) -> p a b", b=inner)
            return ap

        c_act = small[:, 0:16]
        n1w = small[:, 16:32]
        hw = small[:, 32:48]
        cw = small[:, 48:96].rearrange("p (a b) -> p a b", b=3)
        mod_fm = small[:, 96:128]
        A_fm = small[:, 128:144]
        maskt = small[:, 144:145]
        egc = small[:, 152:160]
        ssx = small[:, 160:176]
        rsx = small[:, 176:192]
        bifb = small[:, 192:208]
        uh = small[:, 208:212].rearrange("p (a b) -> p a b", b=2)
        rr = small[:, 224:256]
        g_if = sb("g_if", [128, NT, 16], F32)
        g_sp = gates[:, 1, :].rearrange("p (t c) -> p t c", c=8)
        g_a = gates[:, 2, :].rearrange("p (t c) -> p t c", c=8)
        g_ea = gates[:, 3, :].rearrange("p (t c) -> p t c", c=8)
        g_emb = gates[:, 4, :].rearrange("p (t c) -> p t c", c=8)
        g_eg = gates[:, 5, :].rearrange("p (t c) -> p t c", c=8)

        PS = [stack.enter_context(nc.psum_tensor("ps%d" % i, [128, 1024], F32)) for i in range(4)]
        pp = Pool2(PS)

        s_const = P.new_sem("s_const")
        s_ws = [P.new_sem("s_ws%d" % i) for i in range(3)]
        s_x = [P.new_sem("s_x%d" % i) for i in range(2)]
        s_xo = P.new_sem("s_xo")
        s_out = P.new_sem("s_out")
        wfree = [[] for _ in range(3)]
        wstate = {"i": 0}

        def wview(w):
            return w.rearrange("(kc p) n -> p kc n", p=128)

        def wload(w, c0, width, bias=None):
            i = wstate["i"]
            wstate["i"] = (i + 1) % 3
            slot = WS[i]
            for g4 in range(2):
                ev = P.dma("gpsimd", slot[:, 8 * g4:8 * g4 + 8, 0:width],
                           wview(w)[:, 8 * g4:8 * g4 + 8, c0:c0 + width], wfree[i] if g4 == 0 else (), s_ws[i])
            if bias is not None:
                ev = P.dma("gpsimd", slot[0:1, 16, 0:width], bias[0:1, c0:c0 + width], (), s_ws[i])
            return i, slot, ev

        def wrel(i, ev):
            wfree[i] = [ev]

        cev = []
        for dst, src in ((c_act, c_fm[:, :]), (n1w, norm1_fm[:, :]), (hw, hw_fm[:, :]),
                         (small[:, 48:96], convw_fm[:, :]), (maskt, maskv[:, :]),
                         (bifb, bif_b[:, :]), (maskf[:], tri_in[:, :]), (normf_t[:], normf_b[:, :])):
            cev.append(P.dma("sync", dst, src, (), s_const))
        c_loaded_h = cev[-1]
        s_constg = P.new_sem("s_constg")
        P.dma("gpsimd", identb[:], ident_in[:, :], (), s_constg)
        c_loaded_g = P.dma("gpsimd", wif[:, 0:KC, :], wview(w_in)[:, :, OI:OI + 16], (), s_constg)
        c_loaded = [c_loaded_h, c_loaded_g]
        e_m1 = P.dve(REC.memset(onesf[:], 1.0))
        e_m2 = P.dve(REC.memset(onesrow[:], 1.0))
        e_m3 = P.dve(REC.memset(Dst[:], 0.0))
        e_m4 = P.dve(REC.memset(egc, 1.0), c_loaded)
        e_m5 = P.dve(REC.memset(hist[:], 0.0))
        e_m6 = P.dve(REC.tensor_scalar(out=normf_t[:], in0=normf_t[:], scalar1=float(D ** 0.5),
                                               scalar2=None, op0=ALU.mult), c_loaded)
        e_m7 = P.dve(REC.tensor_scalar(out=hw, in0=hw, scalar1=16.0, scalar2=None, op0=ALU.mult),
                     c_loaded)
        cst_epsD = small[:, 256:257]
        cst_eps256 = small[:, 257:258]
        cst_mhalf = small[:, 258:259]
        e_m8 = P.dve(REC.memset(cst_epsD, float(D * EPS)))
        e_m9 = P.dve(REC.memset(cst_eps256, float(256 * EPS)))
        e_m10 = P.dve(REC.memset(cst_mhalf, -0.5))
        const_ev = c_loaded + [e_m1, e_m2, e_m3, e_m4, e_m5, e_m6, e_m7, e_m8, e_m9, e_m10]

        def pool_rsqrt(dst, src, cst, waits):
            e1 = P.pool(REC.tensor_tensor(out=dst, in0=src, in1=cst, op=ALU.add), list(waits) + const_ev)
            return P.pool(REC.tensor_tensor(out=dst, in0=dst, in1=cst_mhalf, op=ALU.pow), [e1])

        sgc = rr[:, 0:16]
        e = P.act(REC.activation(out=sgc, in_=c_act, func=AF.Sigmoid), c_loaded)
        e_cact = P.dve(REC.tensor_tensor(out=c_act, in0=c_act, in1=sgc, op=ALU.mult), [e] + c_loaded)
        cact_b = TV(0, 2048, F32, 128)
        evs = []
        for kc in range(KC):
            evs.append(P.dve(REC.tensor_copy(
                out=cact_b[:, kc, :], in_=c_act[:, kc:kc + 1].to_broadcast([128, 128])), [e_cact]))
        e_cb = evs[-1]
        s_ada = [P.new_sem("s_ada0"), P.new_sem("s_ada1")]
        ada_free = [[], []]
        mod_ps_i, mod_ps, mod_free = pp.get()
        last_mod_pe = None
        ada_slots = [R2[:, 0:4096].rearrange("p (a b) -> p a b", b=256),
                     R2[:, 4096:8192].rearrange("p (a b) -> p a b", b=256)]
        ada_bias = TV(2048, 512, F32, 256)
        e_mod = None
        for blk in range(24):
            if blk == 16:
                e_mod = P.dve(REC.tensor_copy(out=mod_fm, in_=mod_ps[:, 0:32]), [last_mod_pe])
                pp.rel(mod_ps_i, e_mod)
            si = blk % 2
            slot = ada_slots[si]
            c0 = blk * 256
            for g4 in range(4):
                ev = P.dma("sync", slot[:, 4 * g4:4 * g4 + 4, :], wview(w_ada)[:, 4 * g4:4 * g4 + 4, c0:c0 + 256],
                           ada_free[si] if g4 == 0 else (), s_ada[si])
            ev = P.dma("sync", ada_bias[0:1, si, :], b_ada[0:1, c0:c0 + 256], (), s_ada[si])
            if blk < 16:
                for j in range(2):
                    col = blk * 2 + j
                    for kc in range(KC):
                        P.pe(REC.matmul(
                            mod_ps[:, col:col + 1], lhsT=slot[:, kc, j * 128:(j + 1) * 128],
                            rhs=c_act[:, kc:kc + 1], start=(kc == 0), stop=False),
                            [ev, e_cact] + const_ev + mod_free if kc == 0 else ())
                    last = P.pe(REC.matmul(
                        mod_ps[:, col:col + 1], lhsT=ada_bias[0:1, si, j * 128:(j + 1) * 128],
                        rhs=onesf[0:1, 0:1], start=False, stop=True), (), sig=True)
                ada_free[si] = [last]
                last_mod_pe = last
            else:
                gi, gps, gfree = pp.get()
                gc0 = c0 - 4096
                for kc in range(KC):
                    P.pe(REC.matmul(
                        gps[:, 0:256], lhsT=cact_b[:, kc, :], rhs=slot[:, kc, :],
                        start=(kc == 0), stop=False), [ev, e_cb] + gfree if kc == 0 else ())
                last = P.pe(REC.matmul(
                    gps[:, 0:256], lhsT=onesf[0:1, :], rhs=ada_bias[0:1, si, :],
                    start=False, stop=True), (), sig=True)
                ada_free[si] = [last]
                ec = P.act(REC.activation(
                    out=gate_b[:, gc0:gc0 + 256], in_=gps[:, 0:256], func=AF.Identity), [last])
                pp.rel(gi, ec)
        e_ada_done = last
        e_A = P.dve(REC.scalar_tensor_tensor(
            out=A_fm, in0=mod_fm[:, 16:32], scalar=1.0, in1=n1w, op0=ALU.add, op1=ALU.mult), [e_mod] + c_loaded)
        e_A = P.dve(REC.tensor_scalar(out=A_fm, in0=A_fm, scalar1=float(D ** 0.5), scalar2=None,
                                              op0=ALU.mult), [e_A])
        shift_fm = mod_fm[:, 0:16]
        mod_ready = [e_A, e_mod]

        st = {"R1_free": [], "R2_free": [e_ada_done], "TR_free": [e_cb, e_ada_done],
              "mbT_free": [], "out_evs": []}

        def stage_A(xsrc, ntiles, dst, waits_dst, tr_waits):
            xt = [TV(0, 2048, F32), TV(2048, 2048, F32)]
            xn = [TV(4096, 1024, BF16), TV(5120, 1024, BF16)]
            xfree = [list(tr_waits), list(tr_waits)]
            nfree = [list(tr_waits), list(tr_waits)]
            done = []
            for t in range(ntiles):
                bi = t % 2
                e_ld = P.dma("sync", xt[bi], xsrc[t * 128:(t + 1) * 128, :], xfree[bi], s_x[bi])
                ssc = ssx[:, bi:bi + 1]
                rsc = rsx[:, bi:bi + 1]
                e_sq = P.act(REC.activation(
                    out=xn[bi], in_=xt[bi], func=AF.Square, accum_out=ssc), [e_ld] + nfree[bi])
                e_r2 = pool_rsqrt(rsc, ssc, cst_epsD, [e_sq] + xfree[bi])
                e_xn = P.dve(REC.tensor_scalar(
                    out=xn[bi], in0=xt[bi], scalar1=rsc, scalar2=None, op0=ALU.mult), [e_r2, e_sq, e_ld])
                xfree[bi] = [e_xn]
                pi, ps, pfree = pp.get()
                psb = ps[:].bitcast(BF16).rearrange("p (a b) -> p a b", b=128)
                for c in range(KC):
                    e_tr = P.pe(REC.transpose(
                        out=psb[:, c, :], in_=xn[bi][:, c * 128:(c + 1) * 128], identity=identb[:]),
                        [e_xn] + pfree + const_ev if c == 0 else (), sig=(c == KC - 1))
                nfree[bi] = [e_tr]
                evs = []
                for c in range(KC):
                    o = dst[:, c, t * 128:(t + 1) * 128]
                    if c < KC // 2:
                        evs.append(P.act(REC.activation(
                            out=o, in_=psb[:, c, :], func=AF.Identity,
                            scale=A_fm[:, c:c + 1], bias=shift_fm[:, c:c + 1]),
                            [e_tr] + mod_ready + waits_dst, sig=(c in (KC // 2 - 1, KC - 1))))
                    else:
                        evs.append(P.dve(REC.tensor_scalar(
                            out=o, in0=psb[:, c, :], scalar1=A_fm[:, c:c + 1], scalar2=shift_fm[:, c:c + 1],
                            op0=ALU.mult, op1=ALU.add), [e_tr] + mod_ready + waits_dst, sig=(c in (KC // 2 - 1, KC - 1))))
                pp.rel(pi, evs[KC - 1], evs[KC // 2 - 1])
                done += [evs[KC - 1], evs[KC // 2 - 1]]
            return done, xfree[0] + xfree[1] + nfree[0] + nfree[1]

        def stage_B(hT_ready, first):
            e_c = None
            if not first:
                e_c = P.dve(REC.tensor_copy(out=egc, in_=g_eg[:, NT - 1, :]), st["gates_last"])
            pi, ps, pfree = pp.get()
            psg = ps[:, 0:NT * 16].rearrange("p (t c) -> p t c", c=16)
            for t in range(NT):
                for kc in range(KC):
                    last = P.pe(REC.matmul(
                        psg[:, t, :], lhsT=hT[:, kc, t * 128:(t + 1) * 128], rhs=wif[:, kc, :],
                        start=(kc == 0), stop=(kc == KC - 1)),
                        hT_ready + pfree + const_ev if (t == 0 and kc == 0) else (),
                        sig=(t == NT - 1 and kc == KC - 1))
            wg = st.get("gates_last", []) + ([e_c] if e_c is not None else [])
            for t in range(NT):
                e1 = P.dve(REC.tensor_tensor(
                    out=g_if[:, t, :], in0=psg[:, t, :], in1=bifb, op=ALU.add), [last] + wg + const_ev)
            e2 = P.act(REC.activation(out=g_sp, in_=g_if[:, :, 8:16], func=AF.Exp, scale=-1.0), [e1] + wg)
            e3 = P.act(REC.activation(out=g_sp, in_=g_sp, func=AF.Ln, bias=1.0), [e2])
            psb_ = ps[:, 512:512 + NT * 8].rearrange("p (t c) -> p t c", c=8)
            psg_ = ps[:, 768:768 + NT * 8].rearrange("p (t c) -> p t c", c=8)
            for t in range(NT):
                P.pe(REC.matmul(psb_[:, t, :], lhsT=maskf[:], rhs=g_sp[:, t, :],
                                             start=True, stop=True), [e3, e1])
                e4 = P.pe(REC.matmul(psg_[:, t, :], lhsT=onesf[:], rhs=g_sp[:, t, :],
                                                  start=True, stop=True), (), sig=(t == NT - 1))
            e5 = P.dve(REC.tensor_tensor(out=g_a, in0=psb_, in1=g_if[:, :, 0:8], op=ALU.add), [e4, e1] + wg)
            e6 = P.act(REC.activation(out=g_ea, in_=g_a, func=AF.Exp), [e5] + wg)
            e7 = P.act(REC.activation(out=g_emb, in_=psb_, func=AF.Exp), [e4, e5] + wg)
            e8 = P.act(REC.activation(out=g_eg, in_=psg_, func=AF.Exp, scale=-1.0), [e4, e5] + wg)
            pp.rel(pi, e5, e8)
            return [e6, e7, e8]

        def fm_group(ps_ap, slot, c_in_slot, act_ap, ntok, first_waits, bias=True):
            last = None
            for h0 in range(0, ntok, 512):
                n = min(512, ntok - h0)
                for kc in range(KC):
                    last = P.pe(REC.matmul(
                        ps_ap[:, h0:h0 + n], lhsT=slot[:, kc, c_in_slot:c_in_slot + 128],
                        rhs=act_ap[:, kc, h0:h0 + n], start=(kc == 0), stop=(not bias and kc == KC - 1)),
                        first_waits if (kc == 0 and h0 == 0) else (),
                        sig=(not bias and kc == KC - 1 and h0 + n >= ntok))
                if bias:
                    last = P.pe(REC.matmul(
                        ps_ap[:, h0:h0 + n], lhsT=slot[0:1, 16, c_in_slot:c_in_slot + 128],
                        rhs=onesrow[0:1, 0:n], start=False, stop=True), (), sig=(h0 + n >= ntok))
            return last

        def tm_group(ps_ap, slot, width, act_tile, first_waits, bias=True):
            last = None
            for kc in range(KC):
                last = P.pe(REC.matmul(
                    ps_ap, lhsT=act_tile(kc), rhs=slot[:, kc, 0:width],
                    start=(kc == 0), stop=(not bias and kc == KC - 1)),
                    first_waits if kc == 0 else (), sig=(not bias and kc == KC - 1))
            if bias:
                last = P.pe(REC.matmul(
                    ps_ap, lhsT=onesrow[0:1, 0:128], rhs=slot[0:1, 16, 0:width],
                    start=False, stop=True), (), sig=True)
            return last

        def prefix_state(hT_ready, gate_ev):
            ktok = TV(0, 2048, BF16, 512)
            vp = TV(2048, 4 * NT * 129, BF16)
            vp = vp.rearrange("p (t h c) -> p t h c", h=4, c=258)
            tr_last = []
            for g in range(2):
                wi, slot, wev = wload(w_in, OK_ + g * 512, 512, b_in)
                evk = []
                for t2 in range(NT // 2):
                    pi, ps, pfree = pp.get()
                    for j in range(2):
                        t = t2 * 2 + j
                        last = tm_group(ps[:, j * 512:(j + 1) * 512], slot, 512,
                                        lambda kc, t=t: hT[:, kc, t * 128:(t + 1) * 128],
                                        [wev] + hT_ready + pfree + const_ev + st["TR_free"] + tr_last)
                    ek = P.act(REC.activation(
                        out=ktok[:, 2 * t2:2 * t2 + 2, :], in_=ps[:].rearrange("p (a b) -> p a b", b=512),
                        func=AF.Identity), [last] + st["TR_free"] + tr_last)
                    pp.rel(pi, ek)
                    evk.append(ek)
                wrel(wi, last)
                chk('pk')
                evv = []
                for j2 in range(2):
                    wi, slot, wev = wload(w_in, OV + g * 1024 + j2 * 512, 512, b_in)
                    for t in range(NT):
                        pi, ps, pfree = pp.get()
                        last = tm_group(ps[:, 0:512], slot, 512,
                                        lambda kc, t=t: hT[:, kc, t * 128:(t + 1) * 128],
                                        [wev] + hT_ready + pfree)
                        es = []
                        for hd in range(2):
                            hh = j2 * 2 + hd
                            head = g * 4 + hh
                            es.append(P.dve(REC.tensor_scalar(
                                out=vp[:, t, hh, 0:256], in0=ps[:, hd * 256:(hd + 1) * 256],
                                scalar1=g_ea[:, t, head:head + 1], scalar2=None, op0=ALU.mult),
                                [last] + gate_ev + st["TR_free"] + tr_last))
                        pp.rel(pi, *es)
                        evv += es
                    wrel(wi, last)
                for hh in range(4):
                    head = g * 4 + hh
                    evv.append(P.dve(REC.tensor_copy(
                        out=vp[:, :, hh, 256], in_=g_ea[:, :, head]), gate_ev + st["TR_free"] + tr_last))
                chk('pv')
                tr_last = []
                for t in range(NT):
                    for hh in range(4):
                        head = g * 4 + hh
                        pi, ps, pfree = pp.get()
                        em = P.pe(REC.matmul(
                            ps[:, 0:257], lhsT=ktok[:, t, hh * 128:(hh + 1) * 128], rhs=vp[:, t, hh, 0:257],
                            start=True, stop=True), evk + evv + pfree, sig=True)
                        sc = egc[:, head:head + 1] if t == 0 else g_eg[:, t - 1, head:head + 1]
                        ed = P.dve(REC.scalar_tensor_tensor(
                            out=Dst[:, head, :], in0=Dst[:, head, :], scalar=sc, in1=ps[:, 0:257],
                            op0=ALU.mult, op1=ALU.add), [em] + gate_ev + st["D_ev"][head])
                        st["D_ev"][head] = [ed]
                        pp.rel(pi, ed)
                        tr_last = [em]
            st["TR_free"] = tr_last
            st["hT_readers"] = tr_last

        def branch_B(hT_ready, first_main):
            cu = TV(0, 2 * 1026, F32, 1026)
            yy = TV(2052, 2 * 1024, F32, 1024)
            stmp = TV(4100, 2 * 1024, F32, 1024)
            trf = st["TR_free"]
            rd_ev = []
            last_pe = None
            for cbp in range(8):
                wi, slot, wev = wload(w_in, OU + cbp * 256, 256, b_in)
                e_u = []
                e_uh = []
                for j in range(2):
                    pi, ps, pfree = pp.get()
                    last = fm_group(ps, slot, j * 128, hT, TT, [wev] + hT_ready + pfree + const_ev)
                    if first_main:
                        pass
                    eu = P.act(REC.activation(out=cu[:, j, 2:1026], in_=ps[:, :], func=AF.Identity),
                               [last] + trf + rd_ev)
                    pp.rel(pi, eu)
                    e_u.append(eu)
                if first_main:
                    pi, ps, pfree = pp.get()
                    for j in range(2):
                        for kc in range(KC):
                            P.pe(REC.matmul(
                                ps[:, 2 * j:2 * j + 2], lhsT=slot[:, kc, j * 128:(j + 1) * 128],
                                rhs=hThalo[:, kc, 126:128], start=(kc == 0), stop=False),
                                st["halo_ready"] + pfree if (kc == 0 and j == 0) else ())
                        last = P.pe(REC.matmul(
                            ps[:, 2 * j:2 * j + 2], lhsT=slot[0:1, 16, j * 128:(j + 1) * 128],
                            rhs=onesrow[0:1, 0:2], start=False, stop=True), (), sig=True)
                    euh = P.dve(REC.tensor_copy(
                        out=uh, in_=ps[:, 0:4].rearrange("p (a b) -> p a b", b=2)), [last] + rd_ev)
                    pp.rel(pi, euh)
                    e_uh = [euh]
                wrel(wi, last)
                wi, slot, wev = wload(w_in, OC + cbp * 256, 256, b_in)
                e_cu = []
                for j in range(2):
                    cb = cbp * 2 + j
                    pi, ps, pfree = pp.get()
                    last = fm_group(ps, slot, j * 128, hT, TT, [wev] + pfree)
                    ec = P.dve(REC.tensor_tensor(
                        out=cu[:, j, 2:1026], in0=ps[:, :], in1=cu[:, j, 2:1026], op=ALU.mult), [last, e_u[j]])
                    pp.rel(pi, ec)
                    if not first_main:
                        eh = P.dve(REC.tensor_copy(out=cu[:, j, 0:2], in_=hist[:, cb, :]),
                                   rd_ev + trf + st["hist_ev"])
                        e_cu.append([ec, eh])
                    else:
                        e_cu.append([ec])
                if first_main:
                    pi, ps, pfree = pp.get()
                    for j in range(2):
                        for kc in range(KC):
                            P.pe(REC.matmul(
                                ps[:, 2 * j:2 * j + 2], lhsT=slot[:, kc, j * 128:(j + 1) * 128],
                                rhs=hThalo[:, kc, 126:128], start=(kc == 0), stop=False),
                                pfree if (kc == 0 and j == 0) else ())
                        last = P.pe(REC.matmul(
                            ps[:, 2 * j:2 * j + 2], lhsT=slot[0:1, 16, j * 128:(j + 1) * 128],
                            rhs=onesrow[0:1, 0:2], start=False, stop=True), (), sig=True)
                    eh1 = P.dve(REC.scalar_tensor_tensor(
                        out=cu[:, :, 0:2], in0=ps[:, 0:4].rearrange("p (a b) -> p a b", b=2),
                        scalar=maskt, in1=uh, op0=ALU.mult, op1=ALU.mult), [last] + e_uh + rd_ev + trf + const_ev)
                    pp.rel(pi, eh1)
                    for j in range(2):
                        e_cu[j].append(eh1)
                wrel(wi, last)
                e_y = []
                e_hist = []
                for j in range(2):
                    cb = cbp * 2 + j
                    y1 = P.dve(REC.tensor_scalar(
                        out=yy[:, j, :], in0=cu[:, j, 2:1026], scalar1=cw[:, cb, 2:3], scalar2=None,
                        op0=ALU.mult), e_cu[j] + rd_ev + const_ev)
                    y2 = P.dve(REC.scalar_tensor_tensor(
                        out=yy[:, j, :], in0=cu[:, j, 1:1025], scalar=cw[:, cb, 1:2], in1=yy[:, j, :],
                        op0=ALU.mult, op1=ALU.add), [y1])
                    y3 = P.dve(REC.scalar_tensor_tensor(
                        out=yy[:, j, :], in0=cu[:, j, 0:1024], scalar=cw[:, cb, 0:1], in1=yy[:, j, :],
                        op0=ALU.mult, op1=ALU.add), [y2])
                    eh = P.dve(REC.tensor_copy(out=hist[:, cb, :], in_=cu[:, j, 1024:1026]),
                               e_cu[j] + st["hist_ev"])
                    e_y.append(y3)
                    e_hist.append(eh)
                wi, slot, wev = wload(w_in, OB + cbp * 256, 256, b_in)
                e_b = []
                for j in range(2):
                    pi, ps, pfree = pp.get()
                    last = fm_group(ps, slot, j * 128, hT, TT, [wev] + pfree)
                    eb = P.dve(REC.tensor_tensor(
                        out=yy[:, j, :], in0=ps[:, :], in1=yy[:, j, :], op=ALU.mult), [last, e_y[j]])
                    pp.rel(pi, eb)
                    e_b.append(eb)
                wrel(wi, last)
                wi, slot, wev = wload(w_in, OZB + cbp * 256, 256, b_in)
                new_rd = []
                for j in range(2):
                    cb = cbp * 2 + j
                    pi, ps, pfree = pp.get()
                    last = fm_group(ps, slot, j * 128, hT, TT, [wev] + pfree)
                    es = P.act(REC.activation(out=stmp[:, j, :], in_=ps[:, :], func=AF.Sigmoid),
                               [last] + rd_ev + trf)
                    e1 = P.dve(REC.tensor_tensor(
                        out=yy[:, j, :], in0=yy[:, j, :], in1=stmp[:, j, :], op=ALU.mult), [es, e_b[j]])
                    e2 = P.dve(REC.tensor_tensor(
                        out=hXT[:, cb, :], in0=ps[:, :], in1=yy[:, j, :], op=ALU.mult),
                        [e1, last] + st["R2_free"])
                    pp.rel(pi, e2, es)
                    new_rd += [e2]
                wrel(wi, last)
                last_pe = last
                rd_ev = new_rd + e_hist
                st["hist_ev"] = e_hist
            st["TR_free"] = rd_ev
            return rd_ev

        def merge_stage(w_proj, og, actT, act_ready, hT_ready, accumulate):
            stmp = TV(0, 4096, F32, 1024)
            trf = st["TR_free"]
            rd = [[], [], [], []]
            outs = []
            last = lastG = None
            for db in range(4):
                wiG, slotG, wevG = wload(w_in, og + db * 512, 512, b_in)
                wiP, slotP, wevP = wload(w_proj, db * 512, 512, None)
                es_l = []
                for j in range(4):
                    pi, psG, pfree = pp.get()
                    lastG = fm_group(psG, slotG, j * 128, hT, TT, [wevG] + hT_ready + pfree + const_ev)
                    es = P.act(REC.activation(out=stmp[:, j, :], in_=psG[:, :], func=AF.Sigmoid),
                               [lastG] + rd[j] + trf)
                    pp.rel(pi, es)
                    es_l.append(es)
                wrel(wiG, lastG)
                for j in range(4):
                    blk = db * 4 + j
                    pi, psY, pfree = pp.get()
                    last = fm_group(psY, slotP, j * 128, actT, TT, [wevP] + act_ready + pfree, bias=False)
                    if not accumulate:
                        eo = P.dve(REC.tensor_tensor(
                            out=mbT[:, blk, :], in0=psY[:, :], in1=stmp[:, j, :], op=ALU.mult),
                            [last, es_l[j]] + st["mbT_free"])
                        pp.rel(pi, eo)
                    else:
                        e1 = P.dve(REC.tensor_tensor(
                            out=stmp[:, j, :], in0=psY[:, :], in1=stmp[:, j, :], op=ALU.mult), [last, es_l[j]])
                        pp.rel(pi, e1)
                        eo = P.dve(REC.tensor_tensor(
                            out=mbT[:, blk, :], in0=stmp[:, j, :], in1=mbT[:, blk, :], op=ALU.add), [e1])
                    rd[j] = [eo]
                    outs.append(eo)
                wrel(wiP, last)
            st["TR_free"] = rd[0] + rd[1] + rd[2] + rd[3]
            return outs, [last, lastG]

        def heads_stage(hT_ready, gate_ev):
            qT = TV(0, 512, BF16)
            kT = TV(512, 512, BF16)
            ktok = TV(1024, 512, BF16, 128)
            vp = TV(1536, NT * 129, BF16, 258)
            sgo = TV(2568, 1024, BF16, 256)
            zsl = TV(3592, 1024, BF16, 1024)
            zsg = TV(4616, 1024, F32)
            PT = [TV(5640, 64, BF16), TV(5704, 64, BF16)]
            Sbf = [TV(5768, 130, BF16), TV(5898, 130, BF16)]
            hs = [TV(6028, 257, F32), TV(6285, 257, F32)]
            hn = [TV(6542, 128, BF16), TV(4616 + 200, 128, BF16)]
            sqjunk = TV(4616, 129, BF16)[:, 0:257]
            trf = st["TR_free"]
            e_hc = [P.dve(REC.memset(hs[i][:, 256:257], float((256 * EPS) ** 0.5)), trf) for i in range(2)]
            prev = list(trf) + e_hc
            R2w = st["R2_free"]
            fin = []
            for h in range(NH):
                if h == 0:
                    qk_pref = [wload(w_in, OQ, 128, b_in), wload(w_in, OK_, 128, b_in)]
                wi, slot, wev = qk_pref[0]
                pi, ps, pfree = pp.get()
                last = fm_group(ps, slot, 0, hT, TT, [wev] + hT_ready + pfree + const_ev)
                e_q = P.act(REC.activation(out=qT, in_=ps[:, :], func=AF.Identity,
                                                          scale=float(128 ** -0.5)), [last] + prev)
                pp.rel(pi, e_q)
                wrel(wi, last)
                wi, slot, wev = qk_pref[1]
                pi, ps, pfree = pp.get()
                last = fm_group(ps, slot, 0, hT, TT, [wev] + pfree)
                e_k = P.dve(REC.tensor_copy(out=kT, in_=ps[:, :]), [last] + prev)
                pp.rel(pi, e_k)
                wrel(wi, last)
                pi, ps, pfree = pp.get()
                psb = ps[:, 0:512].bitcast(BF16).rearrange("p (a b) -> p a b", b=128)
                for t in range(NT):
                    e_tr = P.pe(REC.transpose(
                        out=psb[:, t, :], in_=kT[:, t * 128:(t + 1) * 128], identity=identb[:]),
                        [e_k] + pfree if t == 0 else (), sig=(t == NT - 1))
                e_kt = P.act(REC.activation(out=ktok, in_=psb, func=AF.Identity), [e_tr] + prev)
                pp.rel(pi, e_kt)
                wi, slot, wev = wload(w_in, OV + h * 256, 256, b_in)
                e_v = []
                for t4 in range(NT // 4):
                    pi, ps, pfree = pp.get()
                    for j in range(4):
                        t = t4 * 4 + j
                        last = tm_group(ps[:, j * 256:(j + 1) * 256], slot, 256,
                                        lambda kc, t=t: hT[:, kc, t * 128:(t + 1) * 128], [wev] + pfree)
                    es = []
                    for j in range(4):
                        t = t4 * 4 + j
                        fn = REC.tensor_scalar(
                            out=vp[:, t, 0:256], in0=ps[:, j * 256:(j + 1) * 256],
                            scalar1=g_ea[:, t, h:h + 1], scalar2=None, op0=ALU.mult)
                        es.append(P.dve(fn, [last] + gate_ev + prev))
                    pp.rel(pi, *es)
                    e_v += es
                wrel(wi, last)
                e_v.append(P.dve(REC.tensor_copy(out=vp[:, :, 256], in_=g_ea[:, :, h]), gate_ev + prev))
                wi, slot, wev = wload(w_in, OO + h * 256, 256, b_in)
                e_o = []
                for t4 in range(NT // 4):
                    pi, ps, pfree = pp.get()
                    for j in range(4):
                        t = t4 * 4 + j
                        last = tm_group(ps[:, j * 256:(j + 1) * 256], slot, 256,
                                        lambda kc, t=t: hT[:, kc, t * 128:(t + 1) * 128], [wev] + pfree)
                    eo = P.act(REC.activation(
                        out=sgo[:, t4 * 4:(t4 + 1) * 4, :], in_=ps[:].rearrange("p (a b) -> p a b", b=256),
                        func=AF.Sigmoid), [last] + prev)
                    pp.rel(pi, eo)
                    e_o.append(eo)
                wrel(wi, last)
                wi, slot, wev = wload(w_in, OZA + h * 256, 256, b_in)
                e_z = []
                ezprev = list(prev)
                for j in range(2):
                    pi, ps, pfree = pp.get()
                    last = fm_group(ps, slot, j * 128, hT, TT, [wev] + pfree)
                    es = P.act(REC.activation(out=zsg, in_=ps[:, :], func=AF.Sigmoid), [last] + ezprev)
                    ez = P.dve(REC.tensor_tensor(
                        out=zsl[:, j, :], in0=ps[:, :], in1=zsg, op=ALU.mult), [es, last] + prev)
                    pp.rel(pi, ez)
                    ezprev = [ez]
                    e_z.append(ez)
                wrel(wi, last)
                if h + 1 < NH:
                    qk_pref = [wload(w_in, OQ + (h + 1) * 128, 128, b_in),
                               wload(w_in, OK_ + (h + 1) * 128, 128, b_in)]
                e_s = P.act(REC.activation(out=Sbf[0][:, 0:257], in_=Dst[:, h, :], func=AF.Identity,
                                                   scale=egc[:, h:h + 1]), st["D_ev"][h] + gate_ev + prev + const_ev)
                sbf_ev = [e_s]
                pt_free = [list(prev), list(prev)]
                sbf_free = [list(prev), list(prev)]
                hs_free = [list(prev), list(prev)]
                hn_free = [list(prev), list(prev)]
                pend = None
                head_done = []

                def finish(pd):
                    (t, k, piB, psB, e_num, e_hsq, hn_w) = pd
                    psT = psB[:, 260:388].bitcast(BF16).rearrange("p (a b) -> p a b", b=128)
                    for j in range(2):
                        e_tr = P.pe(REC.transpose(
                            out=psT[:, j, :], in_=hn[k][:, j * 128:(j + 1) * 128], identity=identb[:]),
                            [hn_w] if j == 0 else (), sig=(j == 1))
                    evs = []
                    for j in range(2):
                        ch = 2 * h + j
                        evs.append(P.dve(REC.scalar_tensor_tensor(
                            out=hXT[:, ch, t * 128:(t + 1) * 128], in0=psT[:, j, :], scalar=hw[:, ch:ch + 1],
                            in1=zsl[:, j, t * 128:(t + 1) * 128], op0=ALU.mult, op1=ALU.mult),
                            [e_tr] + e_z + R2w + const_ev))
                    pp.rel(piB, *evs)
                    return [e_tr], evs

                for t in range(NT):
                    k = t % 2
                    piA, psA, pfreeA = pp.get()
                    qs = qT[:, t * 128:(t + 1) * 128]
                    ks = kT[:, t * 128:(t + 1) * 128]
                    e_sc = P.pe(REC.matmul(
                        psA[:, 0:128], lhsT=ks, rhs=qs, start=True, stop=True),
                        [e_q, e_k] + pfreeA, sig=True)
                    e_dc = P.pe(REC.matmul(
                        psA[:, 512:769], lhsT=ktok[:, t, :], rhs=vp[:, t, 0:257], start=True, stop=True),
                        [e_kt] + e_v, sig=True)
                    e_pt = P.dve(REC.tensor_tensor(
                        out=PT[k], in0=psA[:, 0:128], in1=maskf[:], op=ALU.mult), [e_sc] + pt_free[k] + const_ev)
                    sc = egc[:, h:h + 1] if t == 0 else g_eg[:, t - 1, h:h + 1]
                    e_d = P.dve(REC.scalar_tensor_tensor(
                        out=Dst[:, h, :], in0=Dst[:, h, :], scalar=sc, in1=psA[:, 512:769],
                        op0=ALU.mult, op1=ALU.add), [e_dc] + st["D_ev"][h] + sbf_ev + gate_ev)
                    st["D_ev"][h] = [e_d]
                    pp.rel(piA, e_pt, e_d)
                    piB, psB, pfreeB = pp.get()
                    P.pe(REC.matmul(
                        psB[:, 0:257], lhsT=PT[k], rhs=vp[:, t, 0:257], start=True, stop=False),
                        [e_pt] + pfreeB)
                    e_num = P.pe(REC.matmul(
                        psB[:, 0:257], lhsT=qs, rhs=Sbf[k][:, 0:257], start=False, stop=True),
                        sbf_ev, sig=True)
                    pt_free[k] = [e_num]
                    sbf_free[k] = [e_num]
                    if t + 1 < NT:
                        e_s = P.act(REC.activation(
                            out=Sbf[1 - k][:, 0:257], in_=Dst[:, h, :], func=AF.Identity,
                            scale=g_eg[:, t, h:h + 1]), [e_d] + sbf_free[1 - k] + gate_ev)
                        sbf_ev = [e_s]
                    if pend is not None:
                        trs, evs = finish(pend)
                        hn_free[pend[1]] = trs
                        head_done = evs
                    rc = rr[:, (h % 2) * 16 + 2 * t:(h % 2) * 16 + 2 * t + 1]
                    sc2 = rr[:, (h % 2) * 16 + 2 * t + 1:(h % 2) * 16 + 2 * t + 2]
                    e_r0 = P.act(REC.activation(
                        out=rc, in_=psB[:, 256:257], func=AF.Abs), [e_num])
                    e_r = P.dve(REC.tensor_scalar(
                        out=rc, in0=rc, scalar1=g_emb[:, t, h:h + 1], scalar2=None, op0=ALU.max),
                        [e_r0] + gate_ev)
                    e_r = P.dve(REC.reciprocal(out=rc, in_=rc), [e_r])
                    e_hs = P.dve(REC.scalar_tensor_tensor(
                        out=hs[k][:, 0:256], in0=psB[:, 0:256], scalar=rc, in1=sgo[:, t, :],
                        op0=ALU.mult, op1=ALU.mult), [e_r] + e_o + hs_free[k])
                    e_sq = P.act(REC.activation(
                        out=sqjunk, in_=hs[k], func=AF.Square, accum_out=sc2), [e_hs] + e_z)
                    e_r2 = P.pool(REC.tensor_tensor(out=sc2, in0=sc2, in1=cst_mhalf, op=ALU.pow),
                                  [e_sq] + const_ev)
                    e_hn = P.act(REC.activation(
                        out=hn[k], in_=hs[k][:, 0:256], func=AF.Identity, scale=sc2),
                        [e_r2, e_sq] + hn_free[k] + e_z)
                    hs_free[k] = [e_hn]
                    pend = (t, k, piB, psB, e_num, e_sq, e_hn)
                trs, evs = finish(pend)
                head_done = evs
                prev = evs + trs + [e_hn, e_num, e_d]
                fin = evs
            st["TR_free"] = prev
            return fin, prev

        def final_stage(tok0, mb_ready, dead_ev):
            tmp = [TV(0, 512, F32), TV(512, 512, F32)]
            trf = st["TR_free"]
            xe = []
            for t in range(NT):
                dst = (xo_lo if t < 4 else xo_hi)[:, t % 4, :]
                xe.append(P.dma("sync", dst, x_main[tok0 + t * 128:tok0 + (t + 1) * 128, :], dead_ev, s_xo))
            x_all = xe[-1]
            tfree = [list(trf), list(trf)]
            acc_ev = [[] for _ in range(NT)]
            last = None
            for db in range(4):
                wi, slot, wev = wload(w_o, db * 512, 512, None)
                for t in range(NT):
                    xo_t = (xo_lo if t < 4 else xo_hi)[:, t % 4, db * 512:(db + 1) * 512]
                    k = t % 2
                    pi, ps, pfree = pp.get()
                    last = tm_group(ps[:, 0:512], slot, 512,
                                    lambda kc, t=t: mbT[:, kc, t * 128:(t + 1) * 128],
                                    [wev] + mb_ready + pfree + const_ev, bias=False)
                    e1 = P.dve(REC.tensor_tensor(
                        out=tmp[k], in0=ps[:, 0:512], in1=gate_b[:, db * 512:(db + 1) * 512], op=ALU.mult),
                        [last] + tfree[k])
                    pp.rel(pi, e1)
                    e2 = P.dve(REC.tensor_tensor(
                        out=xo_t, in0=tmp[k], in1=xo_t, op=ALU.add), [e1, x_all])
                    tfree[k] = [e2]
                    acc_ev[t] = [e2]
                wrel(wi, last)
            st["mbT_free"] = [last]
            chk('finA')
            outs = []
            for t in range(NT):
                xo_t = (xo_lo if t < 4 else xo_hi)[:, t % 4, :]
                ssc = ssx[:, 2 + (t % 2):3 + (t % 2)]
                junk = TV(1024, 1024, BF16)
                e_sq = P.act(REC.activation(
                    out=junk, in_=xo_t, func=AF.Square, accum_out=ssc), acc_ev[t] + outs[-2:])
                e_r2 = pool_rsqrt(ssc, ssc, cst_epsD, [e_sq])
                e_o = P.dve(REC.scalar_tensor_tensor(
                    out=xo_t, in0=xo_t, scalar=ssc, in1=normf_t[:], op0=ALU.mult, op1=ALU.mult),
                    [e_r2, e_sq] + const_ev)
                e_st = P.dma("sync", out[tok0 + t * 128:tok0 + (t + 1) * 128, :], xo_t, [e_o], s_out)
                outs.append(e_o)
                st["out_last"] = e_st
            st["TR_free"] = tfree[0] + tfree[1]
            st["R1_free"] = [st["out_last"]]
            st["R2_free"] = [st["out_last"]]

        def chk(tag):
            if stop == tag:
                raise _StopBuild()

        def main_emit():
            st["D_ev"] = [[e_m3] for _ in range(NH)]
            st["hist_ev"] = [e_m5]
            st["gates_last"] = []
            first = True
            for s_i in range(n_pre_st):
                hev, trl = stage_A(x_pre[s_i * TT:(s_i + 1) * TT, :], NT, hT,
                                   st["R1_free"] + st.get("hT_readers", []), st["TR_free"])
                st["TR_free"] = trl
                gev = stage_B(hev, first)
                chk('preB')
                first = False
                prefix_state(hev, gev)
                chk('prefix1')
                st["gates_last"] = st["TR_free"] + [x for h in range(NH) for x in st["D_ev"][h]]
            if n_pre_st > 0:
                for h in range(NH):
                    ed = P.dve(REC.tensor_scalar(
                        out=Dst[:, h, :], in0=Dst[:, h, :], scalar1=g_eg[:, NT - 1, h:h + 1], scalar2=maskt,
                        op0=ALU.mult, op1=ALU.mult), st["D_ev"][h] + const_ev)
                    st["D_ev"][h] = [ed]
                st["gates_last"] = st["gates_last"] + [ed]
            hev, trl = stage_A(x_halo, 1, hThalo, [], st["TR_free"])
            st["TR_free"] = trl
            st["halo_ready"] = hev
            chk('halo')
            for s_i in range(n_main_st):
                tok0 = s_i * TT
                hev, trl = stage_A(x_main[tok0:tok0 + TT, :], NT, hT,
                                   st["R1_free"] + st.get("hT_readers", []), st["TR_free"])
                st["TR_free"] = trl
                chk('A')
                gev = stage_B(hev, first and s_i == 0)
                chk('B')
                if s_i == 0 and n_pre_st > 0:
                    e_fix = P.dve(REC.memset(egc, 1.0), gev + st["gates_last"])
                    gev = gev + [e_fix]
                hb_ev = branch_B(hev, s_i == 0)
                chk('brB')
                mb_ev, pe_last = merge_stage(w_pb, OGB, hXT, hb_ev, hev, False)
                st["R2_free"] = st["R2_free"] + [pe_last[0]]
                chk('mergeB')
                ha_ev, tr_ev = heads_stage(hev, gev)
                chk('heads')
                st["gates_last"] = tr_ev + [x for h in range(NH) for x in st["D_ev"][h]]
                mb_ev2, pe_last2 = merge_stage(w_pa, OGA, hXT, ha_ev, hev, True)
                chk('mergeA')
                final_stage(tok0, mb_ev2, pe_last2 + mb_ev2)
                st["hT_readers"] = []
        try:
            chk('ada')
            main_emit()
            P.op("sync", None, [st["out_last"]])
        except _StopBuild:
            lasts = [Ev(P.esem[e], P.esem[e].n) for e in ("scalar", "gpsimd", "vector", "tensor") if P.esem[e].n > 0]
            lasts += [Ev(sm, sm.n) for sm in s_ws if sm.n > 0]
            ed = P.dma("sync", out[0:128, :], gate_b[:], lasts, s_out)
            mbw = mbT[:].rearrange("p a b -> p (a b)").bitcast(F32)
            for r4 in range(4):
                ed = P.dma("sync", out[128 * (r4 + 1):128 * (r4 + 2), :], mbw[:, r4 * 2048:(r4 + 1) * 2048], lasts, s_out)
            ed = P.dma("sync", out[640:768, :], Dst[:].rearrange("p a b -> p (a b)")[:, 0:2048], lasts, s_out)
            ed = P.dma("sync", out[768:896, 0:384], gates[:].rearrange("p a b -> p (a b)"), lasts, s_out)
            for r4 in range(4):
                ed = P.dma("sync", out[896 + 128 * r4:1024 + 128 * r4, :], R1[:, r4 * 2048:(r4 + 1) * 2048], lasts, s_out)
            P.op("sync", None, [ed])

        with nc.Block() as block:
            P.emit(block)
    return nc


def _prep_inputs(x, c, norm1_w, w_ada, b_ada, w_in, b_in, conv_w, headnorm_w,
                 w_proj_a, w_proj_b, w_out, normf_w):
    f = lambda a: np.ascontiguousarray(np.asarray(a, dtype=np.float32))
    x = f(x)
    fm = lambda v: f(np.asarray(v, dtype=np.float32).reshape(KC, 128).T)
    shared = {
        "norm1_fm": fm(norm1_w[0]),
        "hw_fm": fm(headnorm_w[0]),
        "convw_fm": f(np.asarray(conv_w[0], dtype=np.float32).reshape(3, KC, 128).transpose(2, 1, 0).reshape(128, KC * 3)),
        "normf_b": f(np.broadcast_to(np.asarray(normf_w, dtype=np.float32)[None, :], (128, D))),
        "bif_b": f(np.broadcast_to(np.asarray(b_in[0], dtype=np.float32)[None, OI:OI + 16], (128, 16))),
        "ident_in": np.eye(128, dtype=np.float32),
        "tri_in": f(np.triu(np.ones((128, 128), dtype=np.float32))),
        "w_ada": f(w_ada[0]), "b_ada": f(np.asarray(b_ada[0])[None, :]),
        "w_in": f(w_in[0]), "b_in": f(np.asarray(b_in[0])[None, :]),
        "w_proj_a": f(w_proj_a[0]), "w_proj_b": f(w_proj_b[0]), "w_out": f(w_out[0]),
    }
    in_maps = []
    for core in range(8):
        b, hf = core // 2, core % 2
        m = dict(shared)
        m["x_main"] = f(x[b, hf * S_CORE:(hf + 1) * S_CORE])
        m["x_pre"] = f(x[b, 0:S_CORE])
        m["x_halo"] = f(x[b, S_CORE - 128:S_CORE])
        m["maskv"] = np.full((128, 1), float(hf), dtype=np.float32)
        m["c_fm"] = fm(np.asarray(c)[b])
        in_maps.append(m)
    return in_maps


_NC_CACHE = {}


def kernel(x, c, norm1_w, w_ada, b_ada, w_in, b_in, conv_w, headnorm_w,
           w_proj_a, w_proj_b, w_out, normf_w):
    in_maps = _prep_inputs(x, c, norm1_w, w_ada, b_ada, w_in, b_in, conv_w, headnorm_w,
                           w_proj_a, w_proj_b, w_out, normf_w)
    if "nc" not in _NC_CACHE:
        _NC_CACHE["nc"] = build_program()
    nc = _NC_CACHE["nc"]
    res = run_bass_kernel_spmd(nc, in_maps, core_ids=list(range(8)))
    outp = np.empty((4, 4096, D), dtype=np.float32)
    for core in range(8):
        b, hf = core // 2, core % 2
        outp[b, hf * S_CORE:(hf + 1) * S_CORE] = res.results[core]["out"]
    return outp
```
